# Optimizing a Trainium2 kernel written in Bass

```python
import math
import jax, jax.numpy as jnp
from jax import lax
import numpy as np

D_MODEL = 1024
BATCH = 8
SEQ = 2048
DEPTH = 2

ATT_HEADS = 6
HEAD_DIM = 64
ATT_W = ATT_HEADS * HEAD_DIM
ROPE_DIM = HEAD_DIM // 4
ROPE_THETA = 500000.0
MOBA_BLOCK = 256
MOBA_TOPK = 3
Q_BLOCK = 128
SSD_HEADS = 6
SSD_HEAD_DIM = 64
SSD_W = SSD_HEADS * SSD_HEAD_DIM
SSD_GROUPS = 2
SSD_STATE = 128
SSD_CONV = 4
SSD_CHUNK = 128
SSD_CONV_W = SSD_W + 2 * SSD_GROUPS * SSD_STATE
GM_GROUPS = 4
GM_W = 256
GM_CHUNK = 128
D_MIX = ATT_W + SSD_W + GM_W
D_IN = 3 * ATT_W + SSD_W + SSD_CONV_W + SSD_HEADS + 2 * GM_W
D_FF = 2816
N_EXPERTS = 8
TOP_K = 2
D_FF_EXPERT = 3584
N_DENSE = (DEPTH + 1) // 2
N_MOE = DEPTH // 2
ALPHA = (2.0 * DEPTH) ** 0.25
BETA = (8.0 * DEPTH) ** -0.25
NEG = -1e30

kernel_name = "hybrid_moba_ssd_gmlp_moe_deepnorm"


def layer_norm(x, g, b, eps=1e-5):
    xf = x.astype(jnp.float32)
    mu = jnp.mean(xf, -1, keepdims=True)
    var = jnp.mean(jnp.square(xf - mu), -1, keepdims=True)
    return ((xf - mu) * lax.rsqrt(var + eps)).astype(x.dtype) * g + b


def rms_norm(x, g, eps=1e-5):
    xf = x.astype(jnp.float32)
    return (xf * lax.rsqrt(jnp.mean(xf * xf, -1, keepdims=True) + eps)).astype(x.dtype) * g


def rope_tables(positions):
    inv = ROPE_THETA ** (-jnp.arange(0, ROPE_DIM, 2, dtype=jnp.float32) / ROPE_DIM)
    ang = positions.astype(jnp.float32)[..., None] * inv
    return jnp.cos(ang), jnp.sin(ang)


def apply_partial_rope(x, cos, sin):
    xr, xp = x[..., :ROPE_DIM], x[..., ROPE_DIM:]
    x1, x2 = jnp.split(xr, 2, axis=-1)
    c = cos[:, :, None, :].astype(x.dtype)
    s = sin[:, :, None, :].astype(x.dtype)
    return jnp.concatenate([x1 * c - x2 * s, x2 * c + x1 * s, xp], axis=-1)


def moba_attention(q, k, v):
    B_, S, H, D = q.shape
    nb = -(-S // MOBA_BLOCK)
    pad = nb * MOBA_BLOCK - S
    qh = q.transpose(0, 2, 1, 3)
    kh = jnp.pad(k.transpose(0, 2, 1, 3), ((0, 0), (0, 0), (0, pad), (0, 0)))
    vh = jnp.pad(v.transpose(0, 2, 1, 3), ((0, 0), (0, 0), (0, pad), (0, 0)))
    kb = kh.reshape(B_, H, nb, MOBA_BLOCK, D)
    vb = vh.reshape(B_, H, nb, MOBA_BLOCK, D)
    k_mean = jnp.mean(kb, axis=3)
    gate = jnp.einsum('bhsd,bhnd->bhsn', qh, k_mean).astype(jnp.float32)
    q_blk = jnp.arange(S) // MOBA_BLOCK
    past = jnp.arange(nb)[None, :] < q_blk[:, None]
    gate = jnp.where(past, gate, NEG)
    k_sel = min(MOBA_TOPK, nb)
    top_val, top_idx = lax.top_k(gate, k_sel)
    top_ok = top_val > NEG / 2

    nq = S // Q_BLOCK
    def to_blocks(t):
        return t.reshape(B_, H, nq, Q_BLOCK, *t.shape[3:]).swapaxes(1, 2).reshape(B_ * nq, H, Q_BLOCK, *t.shape[3:])
    qs, idx, ok = to_blocks(qh), to_blocks(top_idx), to_blocks(top_ok)
    flat = jnp.arange(B_ * nq, dtype=jnp.int32)
    b_ids, qb_ids = flat // nq, flat % nq
    scale = D ** -0.5
    causal_in_block = None

    def one_block(args):
        qi, idx_i, ok_i, b, j = args
        kb_b, vb_b = kb[b], vb[b]
        k_g = jax.vmap(lambda kk, ii: kk[ii])(kb_b, idx_i)
        v_g = jax.vmap(lambda vv, ii: vv[ii])(vb_b, idx_i)
        s_sel = jnp.einsum('hqd,hqkld->hqkl', qi, k_g).astype(jnp.float32) * scale
        s_sel = jnp.where(ok_i[..., None], s_sel, NEG)
        q_pos = j * Q_BLOCK + jnp.arange(Q_BLOCK)
        own = (j * Q_BLOCK) // MOBA_BLOCK
        k_own, v_own = kb_b[:, own], vb_b[:, own]
        k_pos = own * MOBA_BLOCK + jnp.arange(MOBA_BLOCK)
        s_own = jnp.einsum('hqd,hld->hql', qi, k_own).astype(jnp.float32) * scale
        s_own = jnp.where(k_pos[None, :] <= q_pos[:, None], s_own, NEG)
        s = jnp.concatenate([s_sel.reshape(H, Q_BLOCK, k_sel * MOBA_BLOCK), s_own], axis=-1)
        p = jax.nn.softmax(s, axis=-1).astype(qi.dtype)
        p_sel = p[..., :k_sel * MOBA_BLOCK].reshape(H, Q_BLOCK, k_sel, MOBA_BLOCK)
        p_own = p[..., k_sel * MOBA_BLOCK:]
        return jnp.einsum('hqkl,hqkld->hqd', p_sel, v_g) + jnp.einsum('hql,hld->hqd', p_own, v_own)

    out = lax.map(one_block, (qs, idx, ok, b_ids, qb_ids))
    return out.reshape(B_, nq, H, Q_BLOCK, D).transpose(0, 1, 3, 2, 4).reshape(B_, S, H * D)


def causal_depthwise_conv(x, w, b):
    K, C = w.shape
    y = lax.conv_general_dilated(x, w[:, None, :].astype(x.dtype), window_strides=(1,), padding=[(K - 1, 0)],
                                 dimension_numbers=('NWC', 'WIO', 'NWC'), feature_group_count=C)
    return y + b


def ssd_chunked(x, dt, A, Bm, Cm):
    in_dtype = x.dtype
    B_, S, H, P = x.shape
    nc = S // SSD_CHUNK
    rep = H // SSD_GROUPS
    xf = x.astype(jnp.float32)
    Bh = jnp.repeat(Bm.astype(jnp.float32), rep, axis=2)
    Ch = jnp.repeat(Cm.astype(jnp.float32), rep, axis=2)
    xdt = xf * dt[..., None]
    a = dt * A.astype(jnp.float32)
    def ch(t):
        return t.reshape(B_, nc, SSD_CHUNK, *t.shape[2:])
    xdt, Bh, Ch = ch(xdt), ch(Bh), ch(Ch)
    a_cum = jnp.cumsum(ch(a).transpose(0, 3, 1, 2), axis=-1)
    causal = jnp.tril(jnp.ones((SSD_CHUNK, SSD_CHUNK), bool))
    seg = a_cum[..., :, None] - a_cum[..., None, :]
    Lmat = jnp.exp(jnp.where(causal, seg, -jnp.inf))
    CB = jnp.einsum('bclhn,bcshn->bhcls', Ch, Bh)
    y_diag = jnp.einsum('bhcls,bcshp->bclhp', CB * Lmat, xdt)
    decay_states = jnp.exp(a_cum[..., -1:] - a_cum)
    states = jnp.einsum('bclhn,bhcl,bclhp->bchpn', Bh, decay_states, xdt)
    chunk_decay = jnp.exp(a_cum[..., -1])

    def step(hs, inp):
        st, dec = inp
        return hs * dec[..., None, None] + st, hs

    h0 = jnp.zeros((B_, H, P, SSD_STATE), jnp.float32)
    _, prev = lax.scan(step, h0, (states.transpose(1, 0, 2, 3, 4), chunk_decay.transpose(2, 0, 1)))
    prev = prev.transpose(1, 0, 2, 3, 4)
    y_off = jnp.einsum('bclhn,bchpn,bhcl->bclhp', Ch, prev, jnp.exp(a_cum))
    return (y_diag + y_off).reshape(B_, S, H, P).astype(in_dtype)


def ssd_mixer(z, xbc, dt_raw, conv_w, conv_b, dt_bias, a_log, d_skip, norm_g):
    B_, S, _ = z.shape
    xbc = jax.nn.silu(causal_depthwise_conv(xbc, conv_w, conv_b))
    xs, Bm, Cm = jnp.split(xbc, [SSD_W, SSD_W + SSD_GROUPS * SSD_STATE], axis=-1)
    xs = xs.reshape(B_, S, SSD_HEADS, SSD_HEAD_DIM)
    Bm = Bm.reshape(B_, S, SSD_GROUPS, SSD_STATE)
    Cm = Cm.reshape(B_, S, SSD_GROUPS, SSD_STATE)
    dt = jax.nn.softplus(dt_raw.astype(jnp.float32) + dt_bias.astype(jnp.float32))
    A = -jnp.exp(a_log.astype(jnp.float32))
    y = ssd_chunked(xs, dt, A, Bm, Cm) + d_skip[:, None] * xs
    yz = (y.reshape(B_, S, SSD_W) * jax.nn.silu(z)).astype(jnp.float32)
    yg = yz.reshape(B_, S, SSD_GROUPS, SSD_W // SSD_GROUPS)
    yg = yg * lax.rsqrt(jnp.mean(yg * yg, -1, keepdims=True) + 1e-5)
    return yg.reshape(B_, S, SSD_W).astype(z.dtype) * norm_g


def gmlp_sgu(u, v, ln_g, ln_b, w_s, b_s):
    B_, S, _ = v.shape
    nc = S // GM_CHUNK
    v = layer_norm(v, ln_g, ln_b)
    vc = v.reshape(B_, nc, GM_CHUNK, GM_GROUPS, GM_W // GM_GROUPS)
    w = jnp.where(jnp.tril(jnp.ones((GM_CHUNK, GM_CHUNK), bool)), w_s, 0.0)
    mixed = jnp.einsum('gts,bcsgd->bctgd', w.astype(v.dtype), vc) + b_s.T[None, None, :, :, None]
    return u * mixed.reshape(B_, S, GM_W)


def hybrid_mixer(h, cos, sin, w_in, conv_w, conv_b, dt_bias, a_log, d_skip, ssd_norm_g, att_norm_g,
                 gm_ln_g, gm_ln_b, gm_w_s, gm_b_s, gm_norm_g, w_out):
    B_, S, _ = h.shape
    proj = h @ w_in
    c1 = ATT_W
    c2 = 2 * ATT_W
    c3 = 3 * ATT_W
    c4 = c3 + SSD_W
    c5 = c4 + SSD_CONV_W
    c6 = c5 + SSD_HEADS
    c7 = c6 + GM_W
    q, k, v, z, xbc, dt_raw, gu, gv = jnp.split(proj, [c1, c2, c3, c4, c5, c6, c7], axis=-1)
    q = apply_partial_rope(q.reshape(B_, S, ATT_HEADS, HEAD_DIM), cos, sin)
    k = apply_partial_rope(k.reshape(B_, S, ATT_HEADS, HEAD_DIM), cos, sin)
    v = v.reshape(B_, S, ATT_HEADS, HEAD_DIM)
    att = rms_norm(moba_attention(q, k, v), att_norm_g)
    ssd = ssd_mixer(z, xbc, dt_raw, conv_w, conv_b, dt_bias, a_log, d_skip, ssd_norm_g)
    gm = rms_norm(gmlp_sgu(jax.nn.gelu(gu), jax.nn.gelu(gv), gm_ln_g, gm_ln_b, gm_w_s, gm_b_s), gm_norm_g)
    return jnp.concatenate([att, ssd, gm], axis=-1) @ w_out


def swiglu(h, w_gate, w_up, w_down):
    return (jax.nn.silu(h @ w_gate) * (h @ w_up)) @ w_down


def moe_ffn(h, w_router, e_gate, e_up, e_down):
    logits = (h @ w_router).astype(jnp.float32)
    top_val, top_idx = lax.top_k(logits, TOP_K)
    probs = jax.nn.softmax(top_val, axis=-1)
    gates = jnp.sum(jax.nn.one_hot(top_idx, N_EXPERTS, dtype=jnp.float32) * probs[..., None], axis=-2)
    gates = gates.astype(h.dtype)
    out = jnp.zeros_like(h)
    for e in range(N_EXPERTS):
        out = out + gates[..., e:e + 1] * swiglu(h, e_gate[e], e_up[e], e_down[e])
    return out


def setup_inputs(seed: int = 0) -> dict:
    key = jax.random.key(seed)
    ks = jax.random.split(key, 32)
    f32 = jnp.float32

    def nrm(k, shape, scale):
        return jax.random.normal(k, shape, f32) * scale

    def gain(k, shape):
        return 1.0 + 0.01 * jax.random.normal(k, shape, f32)

    x = nrm(ks[0], (BATCH, SEQ, D_MODEL), 1.0)
    positions = jnp.broadcast_to(jnp.arange(SEQ, dtype=jnp.int32), (BATCH, SEQ))
    ln_in_g = gain(ks[1], (D_MODEL,))
    ln_in_b = nrm(ks[2], (D_MODEL,), 0.01)
    col_scale = jnp.concatenate([jnp.ones((2 * ATT_W,), f32), jnp.full((ATT_W,), BETA, f32),
                                 jnp.ones((D_IN - 3 * ATT_W,), f32)])
    w_in = nrm(ks[3], (DEPTH, D_MODEL, D_IN), D_MODEL ** -0.5) * col_scale
    conv_w = nrm(ks[4], (DEPTH, SSD_CONV, SSD_CONV_W), SSD_CONV ** -0.5)
    conv_b = nrm(ks[5], (DEPTH, SSD_CONV_W), 0.01)
    dt0 = jnp.exp(jax.random.uniform(ks[6], (DEPTH, SSD_HEADS), f32, math.log(1e-3), math.log(1e-1)))
    dt_bias = dt0 + jnp.log(-jnp.expm1(-dt0))
    a_log = jnp.log(jax.random.uniform(ks[7], (DEPTH, SSD_HEADS), f32, 1.0, 16.0))
    d_skip = gain(ks[8], (DEPTH, SSD_HEADS))
    ssd_norm_g = gain(ks[9], (DEPTH, SSD_W))
    att_norm_g = gain(ks[10], (DEPTH, ATT_W))
    gm_ln_g = gain(ks[11], (DEPTH, GM_W))
    gm_ln_b = nrm(ks[12], (DEPTH, GM_W), 0.01)
    gm_w_s = nrm(ks[13], (DEPTH, GM_GROUPS, GM_CHUNK, GM_CHUNK), GM_CHUNK ** -0.5)
    gm_b_s = gain(ks[14], (DEPTH, GM_GROUPS, GM_CHUNK))
    gm_norm_g = gain(ks[15], (DEPTH, GM_W))
    w_out = nrm(ks[16], (DEPTH, D_MIX, D_MODEL), D_MIX ** -0.5 * BETA)
    ln1_g = gain(ks[17], (DEPTH, D_MODEL))
    ln1_b = nrm(ks[18], (DEPTH, D_MODEL), 0.01)
    ln2_g = gain(ks[19], (DEPTH, D_MODEL))
    ln2_b = nrm(ks[20], (DEPTH, D_MODEL), 0.01)
    ffn_w_gate = nrm(ks[21], (N_DENSE, D_MODEL, D_FF), D_MODEL ** -0.5)
    ffn_w_up = nrm(ks[22], (N_DENSE, D_MODEL, D_FF), D_MODEL ** -0.5)
    ffn_w_down = nrm(ks[23], (N_DENSE, D_FF, D_MODEL), D_FF ** -0.5 * BETA)
    router_w = nrm(ks[24], (N_MOE, D_MODEL, N_EXPERTS), D_MODEL ** -0.5)
    moe_w_gate = nrm(ks[25], (N_MOE, N_EXPERTS, D_MODEL, D_FF_EXPERT), D_MODEL ** -0.5)
    moe_w_up = nrm(ks[26], (N_MOE, N_EXPERTS, D_MODEL, D_FF_EXPERT), D_MODEL ** -0.5)
    moe_w_down = nrm(ks[27], (N_MOE, N_EXPERTS, D_FF_EXPERT, D_MODEL), D_FF_EXPERT ** -0.5 * BETA)
    return {"x": x, "positions": positions, "ln_in_g": ln_in_g, "ln_in_b": ln_in_b, "w_in": w_in,
            "conv_w": conv_w, "conv_b": conv_b, "dt_bias": dt_bias, "a_log": a_log, "d_skip": d_skip,
            "ssd_norm_g": ssd_norm_g, "att_norm_g": att_norm_g, "gm_ln_g": gm_ln_g, "gm_ln_b": gm_ln_b,
            "gm_w_s": gm_w_s, "gm_b_s": gm_b_s, "gm_norm_g": gm_norm_g, "w_out": w_out,
            "ln1_g": ln1_g, "ln1_b": ln1_b, "ln2_g": ln2_g, "ln2_b": ln2_b,
            "ffn_w_gate": ffn_w_gate, "ffn_w_up": ffn_w_up, "ffn_w_down": ffn_w_down,
            "router_w": router_w, "moe_w_gate": moe_w_gate, "moe_w_up": moe_w_up, "moe_w_down": moe_w_down}


def reference(x, positions, ln_in_g, ln_in_b, w_in, conv_w, conv_b, dt_bias, a_log, d_skip, ssd_norm_g,
              att_norm_g, gm_ln_g, gm_ln_b, gm_w_s, gm_b_s, gm_norm_g, w_out, ln1_g, ln1_b, ln2_g, ln2_b,
              ffn_w_gate, ffn_w_up, ffn_w_down, router_w, moe_w_gate, moe_w_up, moe_w_down):
    cos, sin = rope_tables(positions)
    h = layer_norm(x, ln_in_g, ln_in_b)
    for i in range(DEPTH):
        mix = hybrid_mixer(h, cos, sin, w_in[i], conv_w[i], conv_b[i], dt_bias[i], a_log[i], d_skip[i],
                           ssd_norm_g[i], att_norm_g[i], gm_ln_g[i], gm_ln_b[i], gm_w_s[i], gm_b_s[i],
                           gm_norm_g[i], w_out[i])
        h = layer_norm(ALPHA * h + mix, ln1_g[i], ln1_b[i])
        if i % 2 == 0:
            f = swiglu(h, ffn_w_gate[i // 2], ffn_w_up[i // 2], ffn_w_down[i // 2])
        else:
            f = moe_ffn(h, router_w[i // 2], moe_w_gate[i // 2], moe_w_up[i // 2], moe_w_down[i // 2])
        h = layer_norm(ALPHA * h + f, ln2_g[i], ln2_b[i])
    return h
```

```python
import numpy as np
from contextlib import ExitStack
import concourse.bass as bass
import concourse.mybir as mybir
from concourse.bass_utils import run_bass_kernel_spmd

F32 = mybir.dt.float32
BF16 = mybir.dt.bfloat16
I32 = mybir.dt.int32
AF = mybir.ActivationFunctionType
ALU = mybir.AluOpType
AX = mybir.AxisListType

DEPTH = 2
ALPHA = (2.0 * DEPTH) ** 0.25
EPS = 1e-5
S_LEN = 2048
D = 1024
NT = 16
D_IN = 2950
D_FF = 2816
D_FFE = 3584
NEXP = 8
BIG = 30000.0


class _Op:
    __slots__ = ("eng", "fn", "deps", "isdma", "lane", "lane_cnt", "need_inc", "cnt", "tag", "iname")


class Sched:
    def __init__(self):
        self.ops = []
        self.state = {}
        self.lane_last = {}
        self.lane_n = {}
        self.fence_deps = {}
        self.tag = ""

    def add(self, eng, fn, r=(), w=(), lane=None):
        idx = len(self.ops)
        op = _Op()
        op.eng = eng
        op.fn = fn
        op.isdma = lane is not None
        op.lane = lane
        op.need_inc = False
        op.cnt = 0
        op.tag = self.tag
        op.iname = None
        deps = dict(self.fence_deps)
        for k in r:
            st = self.state.get(k)
            if st is not None and st[0] is not None:
                deps[st[0]] = True
        for k in w:
            st = self.state.get(k)
            if st is not None:
                if st[0] is not None:
                    deps.setdefault(st[0], False)
                for ri in st[1].values():
                    deps.setdefault(ri, False)
                for ri in st[2]:
                    deps.setdefault(ri, False)
        if lane is not None:
            prev = self.lane_last.get(lane)
            if prev is not None:
                deps[prev] = True
            self.lane_last[lane] = idx
            self.lane_n[lane] = self.lane_n.get(lane, 0) + 1
            op.lane_cnt = 16 * self.lane_n[lane]
        for k in r:
            st = self.state.setdefault(k, [None, {}, []])
            if lane is not None:
                st[2].append(idx)
            else:
                st[1][eng] = idx
        for k in w:
            self.state[k] = [idx, {}, []]
        deps.pop(idx, None)
        op.deps = deps
        self.ops.append(op)
        return idx

    def fence(self):
        last = {}
        for i, op in enumerate(self.ops):
            if op.isdma:
                last[("lane", op.lane)] = i
            else:
                last[op.eng] = i
        self.fence_deps = {i: True for i in last.values()}

    def emit(self, nc, stack):
        ops = self.ops
        for op in ops:
            for d, strong in op.deps.items():
                dop = ops[d]
                if dop.isdma:
                    continue
                if dop.eng == op.eng and not op.isdma:
                    if op.eng == "pe":
                        continue
                dop.need_inc = True
        cnt = {}
        for op in ops:
            if not op.isdma and op.need_inc:
                cnt[op.eng] = cnt.get(op.eng, 0) + 1
                op.cnt = cnt[op.eng]
        engs = ["pe", "act", "dve", "pool", "sp"]
        sems = {e: stack.enter_context(nc.semaphore("s_" + e)) for e in engs}
        lanes = sorted(self.lane_n.keys(), key=str)
        lsems = {l: stack.enter_context(nc.semaphore("l_" + str(l))) for l in lanes}
        per = {e: [] for e in engs}
        for i, op in enumerate(ops):
            per[op.eng].append(op)
        final_lane = {l: 16 * n for l, n in self.lane_n.items()}

        def body(eng_name):
            def f(e):
                waited = {}
                for op in per[eng_name]:
                    for d in sorted(op.deps):
                        strong = op.deps[d]
                        dop = ops[d]
                        if dop.isdma:
                            sem, val = lsems[dop.lane], dop.lane_cnt
                            key = ("l", dop.lane)
                        else:
                            if dop.eng == op.eng and not op.isdma:
                                if op.eng == "pe":
                                    continue
                            sem, val = sems[dop.eng], dop.cnt
                            key = ("e", dop.eng)
                        if waited.get(key, 0) >= val:
                            continue
                        e.wait_ge(sem, val)
                        waited[key] = val
                    ins = op.fn(e)
                    try:
                        op.iname = ins.ins.name
                    except Exception:
                        pass
                    if op.isdma:
                        ins.then_inc(lsems[op.lane], 16)
                    elif op.need_inc:
                        ins.then_inc(sems[op.eng], 1)
                if eng_name == "sp":
                    for l in lanes:
                        e.wait_ge(lsems[l], final_lane[l])
            return f

        with nc.Block() as block:
            block.tensor(body("pe"))
            block.scalar(body("act"))
            block.vector(body("dve"))
            block.gpsimd(body("pool"))
            block.sync(body("sp"))


class Builder:
    def __init__(self, n_layers=DEPTH, dbg=(), stop=None):
        self.nc = bass.Bass("TRN2", target_bir_lowering=False)
        self.S = Sched()
        self.stack = ExitStack()
        self.n_layers = n_layers
        self.dbg = set(dbg)
        self.stop = stop
        self.dbg_outs = []
        self.lane_rr = 0
        self.tcount = 0

    def dram_in(self, name, shape, dt=F32):
        return self.nc.dram_tensor(name, list(shape), dt, kind="ExternalInput").ap()

    def sb(self, name, shape, dt):
        return self.stack.enter_context(self.nc.sbuf_tensor(name, list(shape), dt))

    def splane(self):
        self.lane_rr = (self.lane_rr + 1) % 8
        return "sp%d" % self.lane_rr

    def mm(self, out, lhsT, rhs, start=True, stop=True, r=(), w=()):
        self.S.add("pe", lambda e: e.matmul(out, lhsT, rhs, start=start, stop=stop), r, w)

    def tr(self, out, in_, ident, r=(), w=()):
        self.S.add("pe", lambda e: e.transpose(out, in_, ident), r, w)

    def act(self, out, in_, func, bias=None, scale=None, accum=None, r=(), w=()):
        kw = {}
        if bias is not None:
            kw["bias"] = bias
        if scale is not None:
            kw["scale"] = scale
        if accum is not None:
            kw["accum_out"] = accum
        self.S.add("act", lambda e: e.activation(out=out, in_=in_, func=func, **kw), r, w)

    def tt(self, eng, out, in0, in1, op, r=(), w=()):
        self.S.add(eng, lambda e: e.tensor_tensor(out=out, in0=in0, in1=in1, op=op), r, w)

    def ts(self, eng, out, in0, s1, s2, op0, op1=None, r=(), w=()):
        if op1 is None:
            self.S.add(eng, lambda e: e.tensor_scalar(out=out, in0=in0, scalar1=s1, scalar2=None, op0=op0), r, w)
        else:
            self.S.add(eng, lambda e: e.tensor_scalar(out=out, in0=in0, scalar1=s1, scalar2=s2, op0=op0, op1=op1), r, w)

    def stt(self, eng, out, in0, scalar, in1, op0, op1, r=(), w=()):
        self.S.add(eng, lambda e: e.scalar_tensor_tensor(out=out, in0=in0, scalar=scalar, in1=in1, op0=op0, op1=op1), r, w)

    def cp(self, eng, out, in_, r=(), w=()):
        if eng == "act":
            self.S.add("act", lambda e: e.copy(out=out, in_=in_), r, w)
        else:
            self.S.add(eng, lambda e: e.tensor_copy(out=out, in_=in_), r, w)

    def memset(self, eng, ap, val, w=()):
        self.S.add(eng, lambda e: e.memset(ap, val), (), w)

    def dma(self, out, in_, r=(), w=(), eng="sp", lane=None):
        if lane is None:
            lane = self.splane()
        self.S.add(eng, lambda e: e.dma_start(out=out, in_=in_), r, w, lane=lane)

    def dump(self, name, ap, r):
        if name not in self.dbg:
            return
        t = self.nc.dram_tensor("dbg_" + name, list(ap.shape), ap.dtype, kind="ExternalOutput").ap()
        self.dbg_outs.append("dbg_" + name)
        self.dma(t, ap, r=r, w=[("dbgout", name)])

    def pkeys(self, bank, c0=0, c1=512):
        return [("ps", bank)]

    def xtk(self, t):
        return [("XT", t, kc) for kc in range(8)]

    def arena_reset(self):
        self.a_off = 0

    def aalloc(self, shape, dt, at=None):
        n = int(np.prod(shape[1:]))
        nb = n * (2 if dt == BF16 else 4)
        nb = (nb + 3) // 4 * 4
        if at is None:
            off = self.a_off
            self.a_off += nb
        else:
            off = at
        self.last_off = off
        assert self.a_off <= self.ARENA_BYTES, (self.a_off, self.ARENA_BYTES)
        v = self.arena[0:shape[0], off // 2:(off + nb) // 2]
        if dt != BF16:
            v = v.bitcast(dt)
        v = v[:, 0:n]
        if len(shape) == 3:
            v = v.rearrange("p (a b) -> p a b", a=shape[1])
        elif len(shape) == 4:
            v = v.rearrange("p (a b c) -> p a b c", a=shape[1], b=shape[2])
        return v

    def build(self):
        nc = self.nc
        L = self.n_layers
        self.x = self.dram_in("x", [S_LEN, D])
        self.pos = self.dram_in("pos", [128, NT], I32)
        self.cst = self.dram_in("cst", [128, 848])
        self.oneh = self.dram_in("oneh", [8, 8 * 128])
        self.ln_in_g = self.dram_in("ln_in_g", [D])
        self.ln_in_b = self.dram_in("ln_in_b", [D])
        self.w_in = self.dram_in("w_in", [DEPTH, D, D_IN])
        self.conv_wT = self.dram_in("conv_wT", [DEPTH, 128, 7, 4])
        self.conv_bT = self.dram_in("conv_bT", [DEPTH, 128, 7])
        self.dt_bias = self.dram_in("dt_bias", [DEPTH, 6])
        self.a_log = self.dram_in("a_log", [DEPTH, 6])
        self.d_skip = self.dram_in("d_skip", [DEPTH, 6])
        self.normg = self.dram_in("normg", [DEPTH, D])
        self.gm_ln_g = self.dram_in("gm_ln_g", [DEPTH, 256])
        self.gm_ln_b = self.dram_in("gm_ln_b", [DEPTH, 256])
        self.gm_w_s = self.dram_in("gm_w_s", [DEPTH, 4, 128, 128])
        self.gm_b_sT = self.dram_in("gm_b_sT", [DEPTH, 128, 4])
        self.w_out = self.dram_in("w_out", [DEPTH, D, D])
        self.ln1_g = self.dram_in("ln1_g", [DEPTH, D])
        self.ln1_b = self.dram_in("ln1_b", [DEPTH, D])
        self.ln2_g = self.dram_in("ln2_g", [DEPTH, D])
        self.ln2_b = self.dram_in("ln2_b", [DEPTH, D])
        self.ffn_wg = self.dram_in("ffn_w_gate", [1, D, D_FF])
        self.ffn_wu = self.dram_in("ffn_w_up", [1, D, D_FF])
        self.ffn_wd = self.dram_in("ffn_w_down", [1, D_FF, D])
        self.router_w = self.dram_in("router_w", [1, D, NEXP])
        self.moe_wg = self.dram_in("moe_w_gate", [1, NEXP, D, D_FFE])
        self.moe_wu = self.dram_in("moe_w_up", [1, NEXP, D, D_FFE])
        self.moe_wd = self.dram_in("moe_w_down", [1, NEXP, D_FFE, D])
        self.y = nc.dram_tensor("y", [S_LEN, D], F32, kind="ExternalOutput").ap()
        self.w_in_bf = nc.dram_tensor("w_in_bf", [DEPTH, D, D_IN], BF16, kind="Internal").ap()

        self.h_acc = self.sb("h_acc", [128, NT, D], F32)
        self.XT = self.sb("XT", [128, 8, S_LEN], BF16)
        self.cstf = self.sb("cstf", [128, 848], F32)
        self.cstb = self.sb("cstb", [128, 256], BF16)
        self.lnst2 = [self.sb("lnst%d" % i, [128, 2, 6], F32) for i in range(2)]
        self.lnmv2 = [self.sb("lnmv%d" % i, [128, 4], F32) for i in range(2)]
        self.lnst, self.lnmv = self.lnst2[0], self.lnmv2[0]
        self.cs = self.sb("cs", [128, 2, NT, 8], F32)
        self.ARENA_BYTES = 109408
        self.arena = self.sb("arena", [128, self.ARENA_BYTES // 2], BF16)
        p_ = {}
        for i in (0, 1, 4, 5):
            p_[i] = self.stack.enter_context(nc.psum_tensor("ps%d" % i, [128, 512], F32))
        self.ps23 = self.stack.enter_context(nc.psum_tensor("ps23", [128, 1024], F32))
        self.ps67 = self.stack.enter_context(nc.psum_tensor("ps67", [128, 1024], F32))
        p_[2], p_[3] = self.ps23[:, 0:512], self.ps23[:, 512:1024]
        p_[6], p_[7] = self.ps67[:, 0:512], self.ps67[:, 512:1024]
        self.psum = [p_[i] for i in range(8)]

        self.ident_f = self.cstf[:, 0:128]
        self.U_f = self.cstf[:, 128:256]
        self.negm_f = self.cstf[:, 256:384]
        self.tril_f = self.cstf[:, 384:512]
        self.ones_f = self.cstf[:, 512:640]
        self.pastneg = self.cstf[:, 640:704].rearrange("p (b n) -> p b n", b=8)
        self.invf = self.cstf[:, 704:712]
        self.ident_b = self.cstb[:, 0:128]
        self.tri_b = self.cstb[:, 128:256]

        self.dma(self.cstf[:, :], self.cst[:, :], w=["cstf"])
        self.dma(self.cstb[:, 0:128], self.cst[:, 0:128], w=["cstb0"], eng="pool", lane="pw0")
        self.dma(self.cstb[:, 128:256], self.cst[:, 128:256], w=["cstb1"], eng="pool", lane="pw1")
        self.CK = ["cstf", "cstb0", "cstb1"]

        self.S.tag = "init"
        def convert_w_in(l_):
            for r_ in range(4):
                self.dma(self.w_in_bf[l_, r_ * 256:(r_ + 1) * 256, :], self.w_in[l_, r_ * 256:(r_ + 1) * 256, :],
                         w=[("w_in_bf", l_)], eng="pool", lane="cv%d" % r_)
        convert_w_in(0)
        self.rope_tables()
        self.load_ln_params(self.ln_in_g, self.ln_in_b)
        for t in range(NT):
            self.dma(self.h_acc[:, t, :], self.x[t * 128:(t + 1) * 128, :], w=[("h", t)])
        pend = None
        for t in range(NT):
            pn = self.ln_tile(t, final=False, defer=True)
            if pend is not None:
                pend()
            pend = pn
        if pend is not None:
            pend()
        self.S.fence()
        for l_ in range(1, L):
            convert_w_in(l_)
        self.dump("h0", self.h_acc[:, 0:2, :], r=[("h", 0), ("h", 1)])
        self.dump("XT0", self.XT[:, :, 0:256], r=self.xtk(0) + self.xtk(1))
        if self.stop == "ln_in":
            return self.finish()

        for l in range(L):
            self.S.tag = "mixer%d" % l
            self.mixer(l)
            if self.stop in ("proj", "ssd", "att"):
                return self.finish()
            if self.stop == "mixer%d" % l:
                return self.finish()
            self.S.fence()
            self.S.tag = "wout%d" % l
            self.wout_ln1(l)
            if self.stop == "ln1_%d" % l:
                return self.finish()
            self.S.tag = "ffn%d" % l
            if l % 2 == 0:
                self.ffn(l)
            else:
                self.moe(l)
            self.S.tag = "ln2_%d" % l
            last = (l == L - 1) and self.stop is None
            self.load_ln_params(self.ln2_g[l, :], self.ln2_b[l, :])
            pend = None
            for t in range(NT):
                pn = self.ln_tile(t, final=last, need_xt=(not last), defer=True)
                if pend is not None:
                    pend()
                pend = pn
            if pend is not None:
                pend()
            if self.stop == "ln2_%d" % l:
                return self.finish()
            self.S.fence()
        return self.finish()

    def finish(self):
        if self.stop is not None:
            for t in range(NT):
                self.dma(self.y[t * 128:(t + 1) * 128, :], self.h_acc[:, t, :], r=[("h", t)], w=[("y", t)])
        self.S.emit(self.nc, self.stack)
        self.stack.close()
        return self.nc

    def rope_tables(self):
        self.arena_reset()
        self.PB = self.aalloc([128, 2, D], F32)
        self.hb2 = [self.aalloc([128, D], BF16) for _ in range(2)]
        posi = self.aalloc([128, NT], I32)
        posf = self.aalloc([128, NT], F32)
        ang = self.aalloc([128, NT, 8], F32)
        tmp = self.aalloc([128, NT, 8], F32)
        self.dma(posi[:, :], self.pos[:, :], w=["posi"])
        self.cp("dve", posf[:, :], posi[:, :], r=["posi"], w=["posf"])
        self.tt("dve", ang[:, :, :], posf[:, :].unsqueeze(2).to_broadcast([128, NT, 8]),
                self.invf.unsqueeze(1).to_broadcast([128, NT, 8]), ALU.mult, r=["posf", "cstf"], w=["ang"])
        ki = self.aalloc([128, NT, 8], I32)
        kf = self.aalloc([128, NT, 8], F32)
        for which, shift in ((0, 0.75), (1, 0.5)):
            self.ts("dve", tmp[:, :, :], ang[:, :, :], float(1.0 / (2.0 * np.pi)), float(shift), ALU.mult, ALU.add, r=["ang"], w=["angt"])
            self.cp("dve", ki[:, :, :], tmp[:, :, :], r=["angt"], w=["angk"])
            self.cp("dve", kf[:, :, :], ki[:, :, :], r=["angk"], w=["angkf"])
            self.tt("dve", tmp[:, :, :], tmp[:, :, :], kf[:, :, :], ALU.subtract, r=["angt", "angkf"], w=["angt"])
            self.ts("dve", kf[:, :, :], tmp[:, :, :], 1.0, None, ALU.is_ge, r=["angt"], w=["angkf"])
            self.tt("dve", tmp[:, :, :], tmp[:, :, :], kf[:, :, :], ALU.subtract, r=["angt", "angkf"], w=["angt"])
            self.ts("dve", kf[:, :, :], tmp[:, :, :], 0.0, None, ALU.is_lt, r=["angt"], w=["angkf"])
            self.tt("dve", tmp[:, :, :], tmp[:, :, :], kf[:, :, :], ALU.add, r=["angt", "angkf"], w=["angt"])
            self.ts("dve", tmp[:, :, :], tmp[:, :, :], -0.5, float(2.0 * np.pi), ALU.add, ALU.mult, r=["angt"], w=["angt"])
            self.ts("dve", tmp[:, :, :], tmp[:, :, :], float(-np.pi), float(np.pi), ALU.max, ALU.min, r=["angt"], w=["angt"])
            self.act(self.cs[:, which, :, :], tmp[:, :, :], AF.Sin, r=["angt"], w=[("cs", which)])

    def load_ln_params(self, g, b):
        self.dma(self.PB[:, 0, :], g.partition_broadcast(128), w=["PBg"])
        self.dma(self.PB[:, 1, :], b.partition_broadcast(128), w=["PBb"])

    def ln_tile(self, t, final, need_xt=True, defer=False):
        src = self.h_acc[:, t, :]
        hk = ("h", t)
        q_ = t % 2
        st, mv, hb = self.lnst2[q_], self.lnmv2[q_], self.hb2[q_]
        kst, kmv, krs, knm, khb = ("lnst", q_), ("lnmv", q_), ("lnrs", q_), ("lnnm", q_), ("hb", q_)
        for c in range(2):
            self.S.add("dve", (lambda c: lambda e: e.bn_stats(out=st[:, c, :], in_=src[:, c * 512:(c + 1) * 512]))(c),
                       r=[hk], w=[(kst, c)])
        self.S.add("dve", lambda e: e.bn_aggr(out=mv[:, 0:2], in_=st[:, :, :]), r=[(kst, 0), (kst, 1)], w=[kmv])
        self.ts("dve", mv[:, 2:3], mv[:, 1:2], EPS, None, ALU.add, r=[kmv], w=[krs])
        self.tt("pool", mv[:, 2:3], mv[:, 2:3], self.cstf[:, 712:713], ALU.pow, r=[krs, "cstf"], w=[krs])
        self.stt("dve", mv[:, 3:4], mv[:, 0:1], -1.0, mv[:, 2:3], ALU.mult, ALU.mult, r=[kmv, krs], w=[knm])
        self.act(src, src, AF.Identity, bias=mv[:, 3:4], scale=mv[:, 2:3], r=[hk, krs, knm], w=[hk])
        self.tt("dve", src, src, self.PB[:, 0, :], ALU.mult, r=[hk, "PBg"], w=[hk])
        self.tt("dve", src, src, self.PB[:, 1, :], ALU.add, r=[hk, "PBb"], w=[hk])
        if need_xt:
            self.cp("act", hb[:, :], src, r=[hk], w=[khb])
        if final:
            self.dma(self.y[t * 128:(t + 1) * 128, :], src, r=[hk], w=[("y", t)])
        else:
            self.act(src, src, AF.Copy, scale=ALPHA, r=[hk], w=[hk])
        if need_xt:
            if defer:
                return lambda: self.transpose_to_XT(hb, khb, t, range(8))
            self.transpose_to_XT(hb, khb, t, range(8))
        return None

    def transpose_to_XT(self, src_bf, src_key, t, kcs, src_off=0):
        kcs = list(kcs)
        bank = 3 + (self.tcount % 2)
        self.tcount += 1
        pk = ("ps", bank)
        pv = self.psum[bank][:, 0:512].bitcast(BF16).rearrange("p (k n) -> p k n", k=8)
        for i, kc in enumerate(kcs):
            self.tr(pv[:, i, :], src_bf[:, src_off + i * 128:src_off + (i + 1) * 128], self.ident_b,
                    r=[src_key, "cstb0"], w=[pk])
        self.cp("dve", self.XT[:, kcs[0]:kcs[0] + len(kcs), t * 128:(t + 1) * 128], pv[:, 0:len(kcs), :],
                r=[pk], w=[("XT", t, kc) for kc in kcs])

    def gelu_to(self, dst, ps_ap, pk, n):
        sq, w1 = self.g_sq[:, 0:n], self.g_w[:, 0:n]
        self.act(sq, ps_ap, AF.Square, r=[pk], w=["g_sq"])
        self.ts("dve", w1, sq, 0.044715, 1.0, ALU.mult, ALU.add, r=["g_sq"], w=["g_w"])
        self.tt("dve", w1, w1, ps_ap, ALU.mult, r=["g_w", pk], w=["g_w"])
        self.act(sq, w1, AF.Sigmoid, scale=1.5957691216, r=["g_w"], w=["g_sq"])
        return sq

    def mixer(self, l):
        S = self.S
        self.arena_reset()
        A = self.aalloc
        NB = 1 if getattr(self, 'no_overlap', True) else 2
        kT = A([128, 3, 2048], BF16)
        Vaug = A([128, 16, 390], BF16)
        ksum_f = A([128, 3, 8], F32)
        ksum_b = A([128, 3, 8], BF16)
        kn2max = A([128, 6], F32)
        qn2max2 = [A([128, 6], F32) for _ in range(NB)]
        halo = A([128, 7, 3], F32)
        H = A([128, 384], F32)
        Hb = A([128, 384], BF16)
        WsT = A([128, 4, 128], BF16)
        normg = A([128, 1024], F32)
        gmln = A([128, 2, 256], F32)
        cw = A([128, 7, 4], F32)
        cb = A([128, 7], F32)
        bsT = A([128, 4], F32)
        sp6 = A([128, 4, 6], F32)
        NSTG = getattr(self, 'nstg', 2)
        stage = [A([128, 8, 384], BF16) for _ in range(NSTG)]
        qT2 = [A([128, 3, 256], BF16) for _ in range(NB)]
        sz2 = [A([128, 2, 384], F32) for _ in range(NB)]
        xs_tok2 = [A([128, 2, 384], F32) for _ in range(NB)]
        BT2 = [A([128, 2, 256], BF16) for _ in range(NB)]
        CT2 = [A([128, 2, 256], BF16) for _ in range(NB)]
        B_tok2 = [A([128, 2, 2, 128], BF16) for _ in range(NB)]
        att_pre = A([128, 2, 384], F32)
        dtc2 = [A([128, 2, 6], F32) for _ in range(NB)]
        ggu = A([128, 2, 256], F32)
        mixas = A([128, 2, 768], BF16)
        mixgm2 = [A([128, 2, 256], BF16) for _ in range(NB)]
        qf = A([128, 384], F32)
        rt = A([128, 4, 6, 8], F32)
        qbs = {(g_, i_): A([128, 384], BF16) for g_ in ('q', 'k') for i_ in range(2)}
        self.g_sq = A([128, 256], F32)
        sqt = A([128, 384], F32, at=self.last_off)
        self.g_w = A([128, 256], F32)
        ggv2 = A([128, 2, 256], F32)
        gmp2 = A([128, 2, 256], F32)
        vln2 = A([128, 2, 256], BF16)
        raw2 = [A([128, 259], F32) for _ in range(2)]
        Wsb = A([128, 4, 128], BF16, at=self.last_off - 1036)
        cacc2 = [A([128, 256], F32) for _ in range(2)]
        xsT3 = A([128, 3, 256], F32)
        sm = A([128, 128], F32)
        a_bc2 = A([128, 2, 3, 128], F32)
        seg2 = A([128, 2, 3, 128], F32)
        MT2 = A([128, 2, 3, 128], BF16)
        xdt2 = A([128, 2, 384], BF16)
        xdtd2 = A([128, 2, 384], BF16)
        ytmp2 = A([128, 2, 384], F32)
        yo = A([128, 384], F32)
        yy = A([128, 384], F32)
        ET = [A([128, 256], BF16) for _ in range(3)]
        gsel = A([128, 2, 6, 8], F32)
        acc = A([128, 2, 65], F32)
        atmp = A([128, 2, 65], F32)
        gtmp = A([128, 6, 8], F32)
        gtop = A([128, 6, 8], F32)
        st12 = A([128, 16], F32)
        st12b = A([128, 16], F32)
        self.g_lnst = A([128, 2, 6], F32)
        self.g_lnmv = A([128, 4], F32)
        dgt = A([128, 16], F32)
        negM = A([128, 8], F32)
        rden = A([128, 4], F32)
        ssq = A([128, 16], F32)
        PS = self.psum
        CK = "cstf"

        self.dma(normg[:, :], self.normg[l, :].partition_broadcast(128), w=["normg"])
        self.dma(gmln[:, 0, :], self.gm_ln_g[l, :].partition_broadcast(128), w=["gmln0"])
        self.dma(gmln[:, 1, :], self.gm_ln_b[l, :].partition_broadcast(128), w=["gmln1"])
        self.dma(cw[:, :, :], self.conv_wT[l, :, :, :], w=["cw"])
        self.dma(cb[:, :], self.conv_bT[l, :, :], w=["cb"])
        self.dma(bsT[:, :], self.gm_b_sT[l, :, :], w=["bsT"])
        self.dma(sp6[:, 0, :], self.dt_bias[l, :].partition_broadcast(128), w=["dtb"])
        self.dma(sp6[:, 1, :], self.a_log[l, :].partition_broadcast(128), w=["alog"])
        self.dma(sp6[:, 2, :], self.d_skip[l, :].partition_broadcast(128), w=["dsk"])
        self.act(sp6[:, 3, :], sp6[:, 1, :], AF.Exp, r=["alog"], w=["Aneg"])
        self.ts("dve", sp6[:, 3, :], sp6[:, 3, :], -1.0, None, ALU.mult, r=["Aneg"], w=["Aneg"])
        self.dma(Wsb[:, :, :], self.gm_w_s[l, :, :, :].rearrange("g t s -> t g s"), w=[("raw", 0), ("rawh", 0)], eng="pool", lane="pw2")
        self.tt("dve", Wsb[:, :, :], Wsb[:, :, :], self.tril_f.unsqueeze(1).to_broadcast([128, 4, 128]), ALU.mult,
                r=[("raw", 0), ("rawh", 0), CK], w=[("raw", 0), ("rawh", 0)])
        pv = PS[3][:, 0:256].bitcast(BF16).rearrange("p (g n) -> p g n", g=4)
        for g in range(4):
            self.tr(pv[:, g, :], Wsb[:, g, :], self.ident_b, r=[("raw", 0), ("rawh", 0), "cstb0"], w=[("ps", 3)])
        self.cp("dve", WsT[:, :, :], pv, r=[("ps", 3)], w=["WsT"])
        self.memset("pool", Vaug[:, :, :], 1.0, w=[("V", t) for t in range(NT)])
        self.memset("pool", H[:, :], 0.0, w=["H"])
        self.memset("pool", Hb[:, :], 0.0, w=["Hb"])
        self.memset("pool", halo[:, :, :], 0.0, w=[("halo", j) for j in range(7)])
        self.memset("pool", kn2max[:, :], 0.0, w=["kn2max"])
        self.memset("pool", ksum_f[:, :, :], 0.0, w=["ksum_f"])

        groups = [("q", 0, 384), ("k", 384, 384), ("v", 768, 384), ("z", 1152, 384), ("xa", 1536, 384),
                  ("xb", 1920, 384), ("xc", 2304, 134), ("gu", 2438, 256), ("gv", 2694, 256)]
        nchunks = getattr(self, "mixer_chunks", 8)
        stage_n = [0]
        bank_n = [0]

        deferred = []

        def next_bank():
            bank_n[0] += 1
            return bank_n[0] % 2

        cos = self.cs[:, 0, :, :]
        sin = self.cs[:, 1, :, :]

        def rope(buf, key, t):
            v = buf[:, :].rearrange("p (h d) -> p h d", h=6)
            x1, x2 = v[:, :, 0:8], v[:, :, 8:16]
            c = cos[:, t:t + 1, :].to_broadcast([128, 6, 8])
            s_ = sin[:, t:t + 1, :].to_broadcast([128, 6, 8])
            self.tt("dve", rt[:, 0, :, :], x1, c, ALU.mult, r=[key, ("cs", 0)], w=["rt0"])
            self.tt("dve", rt[:, 1, :, :], x2, s_, ALU.mult, r=[key, ("cs", 1)], w=["rt1"])
            self.tt("dve", rt[:, 2, :, :], x2, c, ALU.mult, r=[key, ("cs", 0)], w=["rt2"])
            self.tt("dve", rt[:, 3, :, :], x1, s_, ALU.mult, r=[key, ("cs", 1)], w=["rt3"])
            self.tt("dve", x1, rt[:, 0, :, :], rt[:, 1, :, :], ALU.subtract, r=["rt0", "rt1"], w=[key])
            self.tt("dve", x2, rt[:, 2, :, :], rt[:, 3, :, :], ALU.add, r=["rt2", "rt3"], w=[key])

        def proj_gen(c):
            p = c % NB
            qT, sz, xs_tok, BT, CT, B_tok, dtc, mixp, qn2max = (qT2[p], sz2[p], xs_tok2[p], BT2[p], CT2[p], B_tok2[p],
                                                                dtc2[p], None, qn2max2[p])
            tiles = [2 * c, 2 * c + 1]
            ccols = slice(c * 256, (c + 1) * 256)
            for (gname, c0, ncol) in groups:
                yield
                prev_def = deferred[:]
                del deferred[:]
                self.S.tag = "m%d.c%d.proj_%s" % (l, c, gname)
                sidx = stage_n[0] % NSTG
                stage_n[0] += 1
                stg = stage[sidx]
                sk = ("stage", sidx)
                self.dma(stg[:, :, 0:ncol], self.w_in_bf[l, :, c0:c0 + ncol].rearrange("(kc p) n -> p kc n", p=128),
                         r=[("w_in_bf", l)], w=[sk], eng="sp", lane="st%d" % sidx)
                if gname in ("q", "k", "v", "z", "gu", "gv"):
                    for i, t in enumerate(tiles):
                        if i == 1:
                            yield
                        pb = next_bank()
                        pk = ("ps", pb)
                        ps = PS[pb][:, 0:ncol]
                        for kc in range(8):
                            self.mm(ps, self.XT[:, kc, t * 128:(t + 1) * 128], stg[:, kc, 0:ncol], start=(kc == 0),
                                    stop=(kc == 7), r=[("XT", t, kc), sk], w=[pk])
                        if i == 0:
                            for fn_ in prev_def:
                                fn_()
                            prev_def = []
                        if gname in ("q", "k"):
                            buf, bk = (qf, "qf")
                            self.act(buf[:, :], ps, AF.Copy, scale=(0.125 if gname == "q" else 1.0), r=[pk], w=[bk])
                            rope(buf, bk, t)
                            qb = qbs[(gname, i)]
                            qbk = ("qb", gname, i)
                            self.cp("act", qb[:, :], buf[:, :], r=[bk], w=[qbk])
                            self.tt("dve", sqt[:, :], buf[:, :], buf[:, :], ALU.mult, r=[bk], w=["g_sq", "g_w"])
                            if gname == "q":
                                dstn, dk = st12[:, 0:6], "qn2"
                            else:
                                dstn, dk = st12[:, 8:14], "kn2"
                            S.add("dve", (lambda dstn: lambda e: e.tensor_reduce(
                                out=dstn, in_=sqt[:, :].rearrange("p (h d) -> p h d", h=6), axis=AX.X, op=ALU.add))(dstn),
                                r=["g_sq", "g_w"], w=[dk])
                            if gname == "q":
                                if i == 0:
                                    self.cp("dve", qn2max[:, :], dstn, r=[dk], w=[("qn2max", p)])
                                else:
                                    self.tt("dve", qn2max[:, :], qn2max[:, :], dstn, ALU.max, r=[dk, ("qn2max", p)], w=[("qn2max", p)])
                            else:
                                self.tt("dve", kn2max[:, :], kn2max[:, :], dstn, ALU.max, r=[dk, "kn2max"], w=["kn2max"])

                            def _tr(gname=gname, i=i, t=t, qb=qb, qbk=qbk, c=c, tiles=tiles):
                                tb = 3
                                self.tcount += 1
                                tk = ("ps", tb)
                                tv = PS[tb][:, 0:192].bitcast(BF16).rearrange("p (k n) -> p k n", k=3)
                                for pr in range(3):
                                    self.tr(tv[:, pr, :], qb[:, pr * 128:(pr + 1) * 128], self.ident_b, r=[qbk, "cstb0"], w=[tk])
                                if gname == "q":
                                    self.cp("dve", qT[:, :, i * 128:(i + 1) * 128], tv, r=[tk], w=[("qT", p, i)])
                                else:
                                    self.cp("dve", kT[:, :, t * 128:(t + 1) * 128], tv, r=[tk], w=[("kT", t)])
                                    if i == 1:
                                        S.add("dve", (lambda c: lambda e: e.tensor_reduce(
                                            out=ksum_f[:, :, c], in_=kT[:, :, c * 256:(c + 1) * 256], axis=AX.X, op=ALU.add))(c),
                                            r=[("kT", tiles[0]), ("kT", tiles[1])], w=["ksum_f"])
                                        self.cp("dve", ksum_b[:, :, :], ksum_f[:, :, :], r=["ksum_f"], w=["ksum_b"])
                            deferred.append(_tr)
                        elif gname == "v":
                            vv = Vaug[:, t, :].rearrange("p (h e) -> p h e", e=65)[:, :, 0:64]
                            self.act(vv, ps.rearrange("p (h d) -> p h d", h=6), AF.Copy, r=[pk], w=[("V", t)])
                        elif gname == "z":
                            self.act(sz[:, i, :], ps, AF.Silu, r=[pk], w=[("sz", p, i)])
                        elif gname == "gu":
                            sg = self.gelu_to(None, ps, pk, 256)
                            self.tt("dve", ggu[:, i, :], sg, ps, ALU.mult, r=["g_sq", pk], w=[("ggu", i)])
                        elif gname == "gv":
                            sg = self.gelu_to(None, ps, pk, 256)
                            self.tt("dve", ggv2[:, i, :], sg, ps, ALU.mult, r=["g_sq", pk], w=[("ggv", i)])
                            if i == 1:
                                self.gmlp_chunk(l, ggv2, vln2, gmp2, ggu, WsT, bsT, gmln, normg, mixgm2[p], ssq, p)
                else:
                    njl = {"xa": 3, "xb": 3, "xc": 1}[gname]
                    j0 = {"xa": 0, "xb": 3, "xc": 6}[gname]
                    for jl in range(njl):
                        j = j0 + jl
                        xb_ = 2 if j % 2 == 0 else 4
                        pk = ("ps", xb_)
                        ps = PS[xb_][:, 0:256]
                        for kc in range(8):
                            self.mm(ps, stg[:, kc, jl * 128:(jl + 1) * 128], self.XT[:, kc, ccols], start=(kc == 0),
                                    stop=(kc == 7), r=[("XT", tiles[0], kc), ("XT", tiles[1], kc), sk], w=[pk])
                        if jl == 0:
                            for fn_ in prev_def:
                                fn_()
                            prev_def = []
                        self.conv_chunk(j, ps, pk, raw2[j % 2], halo, cw, cb, cacc2[j % 2], xsT3, xs_tok, BT, CT, B_tok, deferred, p)
                    if gname == "xc":
                        pk = ("ps", 3)
                        for i, t in enumerate(tiles):
                            for kc in range(8):
                                self.mm(PS[3][:, i * 8:i * 8 + 6], self.XT[:, kc, t * 128:(t + 1) * 128], stg[:, kc, 128:134],
                                        start=(kc == 0), stop=(kc == 7), r=[("XT", t, kc), sk], w=[pk])
                        xv = sm[:, 0:12].rearrange("p (i h) -> p i h", i=2)
                        ax = sm[:, 12:24].rearrange("p (i h) -> p i h", i=2)
                        self.tt("dve", xv, PS[3][:, 0:16].rearrange("p (i h) -> p i h", i=2)[:, :, 0:6],
                                sp6[:, 0:1, :].to_broadcast([128, 2, 6]), ALU.add, r=[pk, "dtb"], w=["dtx"])
                        self.stt("dve", ax, xv, -1.0, xv, ALU.mult, ALU.max, r=["dtx"], w=["dtax"])
                        self.act(ax, ax, AF.Exp, scale=-1.0, r=["dtax"], w=["dtax"])
                        self.act(ax, ax, AF.Ln, bias=1.0, r=["dtax"], w=["dtax"])
                        self.stt("dve", dtc[:, :, :], xv, 0.0, ax, ALU.max, ALU.add, r=["dtx", "dtax"], w=[("dtc", p)])
            for fn_ in deferred:
                fn_()
            del deferred[:]

        def back_gen(c):
            p = c % NB
            qT, sz, xs_tok, BT, CT, B_tok, dtc, mixp, qn2max = (qT2[p], sz2[p], xs_tok2[p], BT2[p], CT2[p], B_tok2[p],
                                                                dtc2[p], None, qn2max2[p])
            tiles = [2 * c, 2 * c + 1]
            self.S.tag = "m%d.c%d.ssd" % (l, c)
            yield
            self.ssd_chunk(l, c, p, dict(sm=sm, sp6=sp6, dtc=dtc, xs_tok=xs_tok, BT=BT, CT=CT, B_tok=B_tok, a_bc2=a_bc2,
                                         seg2=seg2, MT2=MT2, xdt2=xdt2, xdtd2=xdtd2, ytmp2=ytmp2, yo=yo, yy=yy, H=H, Hb=Hb, sz=sz,
                                         normg=normg, mixp=mixas, ssq=ssq))
            self.S.tag = "m%d.c%d.att" % (l, c)
            yield from self.attention_chunk(l, c, p, dict(kT=kT, qT=qT, Vaug=Vaug, ksum_b=ksum_b, kn2max=kn2max, qn2max=qn2max, st12=st12,
                                            st12b=st12b, dgt=dgt, negM=negM, ET=ET, gsel=gsel, acc=acc, atmp=atmp, gtmp=gtmp, gtop=gtop,
                                            att_pre=att_pre, rden=rden, ssq=ssq, normg=normg, mixp=mixas, sqt=yo))
            self.S.tag = "m%d.c%d.mixT" % (l, c)
            for i, t in enumerate(tiles):
                yield
                keys = [("mixp", "att", i), ("mixp", "ssd", i), ("mixp", p, "gm", i)]
                bank = 4 + (self.tcount % 2)
                self.tcount += 1
                pk = ("ps", bank)
                pvv = PS[bank][:, 0:512].bitcast(BF16).rearrange("p (k n) -> p k n", k=8)
                for kc in range(8):
                    srcm = mixas[:, i, kc * 128:(kc + 1) * 128] if kc < 6 else mixgm2[p][:, i, (kc - 6) * 128:(kc - 5) * 128]
                    self.tr(pvv[:, kc, :], srcm, self.ident_b, r=keys + ["cstb0"], w=[pk])
                self.cp("dve", self.XT[:, :, t * 128:(t + 1) * 128], pvv, r=[pk], w=self.xtk(t))


        def drain(g):
            for _ in g:
                pass

        def merge(g1, n1, g2, n2):
            a1 = a2 = True
            e = 0.0
            while a1 or a2:
                pick1 = a1 and (not a2 or e <= 0)
                if pick1:
                    try:
                        next(g1)
                        e += float(n2) / max(n1, 1)
                    except StopIteration:
                        a1 = False
                else:
                    try:
                        next(g2)
                        e -= 1.0
                    except StopIteration:
                        a2 = False

        drain(proj_gen(0))
        for c in range(nchunks):
            nb_ = 8 + 6 * (2 * c + 2) + (6 * c if c >= 4 else 0)
            if c + 1 < nchunks and not getattr(self, "no_overlap", True):
                merge(back_gen(c), nb_, proj_gen(c + 1), 19)
            else:
                drain(back_gen(c))
                if c + 1 < nchunks:
                    drain(proj_gen(c + 1))
        if self.stop in ("proj", "ssd", "att"):
            self.dump("mixp", mixas[:, :, :], r=[("mixp", pc, i) for pc in ("att", "ssd") for i in range(2)])
            pl = (nchunks - 1) % NB
            self.dump("mixgm", mixgm2[pl][:, :, :], r=[("mixp", pl, "gm", i) for i in range(2)])
            self.dump("dtc", dtc2[pl][:, :, :], r=[("dtc", pl)])
            return

    def rstd_from(self, dst, src, inv_n, rk, wk):
        self.ts("dve", dst, src, float(inv_n), EPS, ALU.mult, ALU.add, r=rk, w=[wk])
        n = dst.shape[1]
        self.tt("pool", dst, dst, self.cstf[:, 712:713].to_broadcast([128, n]), ALU.pow, r=[wk, "cstf"], w=[wk])

    def gmlp_tile(self, l, i, t, ggv, vln, gmp, ggu, WsT, bsT, gmln, normg, mixp, ssq, p):
        S = self.S
        st, mv = self.g_lnst, self.g_lnmv
        S.add("dve", lambda e: e.bn_stats(out=st[:, 0, :], in_=ggv[:, :]), r=["ggv"], w=["g_lnst"])
        S.add("dve", lambda e: e.bn_aggr(out=mv[:, 0:2], in_=st[:, 0:1, :]), r=["g_lnst"], w=["g_lnmv"])
        self.ts("dve", mv[:, 2:3], mv[:, 1:2], EPS, None, ALU.add, r=["g_lnmv"], w=["g_lnrs"])
        self.tt("pool", mv[:, 2:3], mv[:, 2:3], self.cstf[:, 712:713], ALU.pow, r=["g_lnrs", "cstf"], w=["g_lnrs"])
        self.stt("dve", mv[:, 3:4], mv[:, 0:1], -1.0, mv[:, 2:3], ALU.mult, ALU.mult, r=["g_lnmv", "g_lnrs"], w=["g_lnnm"])
        self.act(ggv[:, :], ggv[:, :], AF.Identity, bias=mv[:, 3:4], scale=mv[:, 2:3], r=["ggv", "g_lnrs", "g_lnnm"], w=["ggv"])
        self.tt("dve", ggv[:, :], ggv[:, :], gmln[:, 0, :], ALU.mult, r=["ggv", "gmln0"], w=["ggv"])
        self.tt("dve", vln[:, :], ggv[:, :], gmln[:, 1, :], ALU.add, r=["ggv", "gmln1"], w=["vln"])
        pk = ("ps", 2)
        ps = self.psum[2][:, 0:256]
        for g in range(4):
            self.mm(ps[:, g * 64:(g + 1) * 64], WsT[:, g, :], vln[:, g * 64:(g + 1) * 64], r=["WsT", "vln"], w=[pk])
        for g in range(4):
            self.stt("dve", gmp[:, g * 64:(g + 1) * 64], ps[:, g * 64:(g + 1) * 64], bsT[:, g:g + 1], ggu[:, i, g * 64:(g + 1) * 64],
                     ALU.add, ALU.mult, r=[pk, "bsT", ("ggu", i)], w=["g_w"])
        self.memset("dve", ssq[:, 0:1], 0.0, w=["ssq0"])
        self.act(ggv[:, :], gmp[:, :], AF.Square, accum=ssq[:, 0:1], r=["g_w", "ssq0"], w=["ggv", "ssq0"])
        self.rstd_from(ssq[:, 1:2], ssq[:, 0:1], 1.0 / 256.0, ["ssq0"], "ssq1")
        self.stt("dve", mixp[:, i, :], gmp[:, :], ssq[:, 1:2], normg[:, 768:1024], ALU.mult, ALU.mult,
                 r=["g_w", "ssq1", "normg"], w=[("mixp", p, "gm", i)])

    def gmlp_chunk(self, l, ggv, vln, gmp, ggu, WsT, bsT, gmln, normg, mixp, ssq, p):
        S = self.S
        st, mv = self.g_lnst, self.g_lnmv
        rs = ssq[:, 8:12]
        for i in range(2):
            S.add("dve", (lambda i: lambda e: e.bn_stats(out=st[:, i, :], in_=ggv[:, i, :]))(i), r=[("ggv", i)], w=[("g_lnst", i)])
            S.add("dve", (lambda i: lambda e: e.bn_aggr(out=mv[:, 2 * i:2 * i + 2], in_=st[:, i:i + 1, :]))(i),
                  r=[("g_lnst", i)], w=["g_lnmv"])
        mvv = mv[:, 0:4].rearrange("p (i s) -> p i s", i=2)
        self.ts("dve", rs[:, 0:2], mvv[:, :, 1], EPS, None, ALU.add, r=["g_lnmv"], w=["g_lnrs"])
        self.tt("pool", rs[:, 0:2], rs[:, 0:2], self.cstf[:, 712:713].to_broadcast([128, 2]), ALU.pow, r=["g_lnrs", "cstf"], w=["g_lnrs"])
        self.stt("dve", rs[:, 2:4], mvv[:, :, 0], -1.0, rs[:, 0:2], ALU.mult, ALU.mult, r=["g_lnmv", "g_lnrs"], w=["g_lnnm"])
        for i in range(2):
            self.act(ggv[:, i, :], ggv[:, i, :], AF.Identity, bias=rs[:, 2 + i:3 + i], scale=rs[:, i:i + 1],
                     r=[("ggv", i), "g_lnrs", "g_lnnm"], w=[("ggv", i)])
        gk = [("ggv", 0), ("ggv", 1)]
        self.tt("dve", ggv[:, :, :], ggv[:, :, :], gmln[:, 0:1, :].to_broadcast([128, 2, 256]), ALU.mult, r=gk + ["gmln0"], w=gk)
        self.tt("dve", vln[:, :, :], ggv[:, :, :], gmln[:, 1:2, :].to_broadcast([128, 2, 256]), ALU.add, r=gk + ["gmln1"], w=["vln"])
        pk = ("ps", 2)
        ps = self.psum[2][:, 0:512].rearrange("p (i n) -> p i n", i=2)
        for i in range(2):
            for g in range(4):
                self.mm(ps[:, i, g * 64:(g + 1) * 64], WsT[:, g, :], vln[:, i, g * 64:(g + 1) * 64], r=["WsT", "vln"], w=[pk])
        for i in range(2):
            self.tt("dve", gmp[:, i, :].rearrange("p (g d) -> p g d", g=4), ps[:, i, :].rearrange("p (g d) -> p g d", g=4),
                    bsT[:, 0:4].unsqueeze(2).to_broadcast([128, 4, 64]), ALU.add, r=[pk, "bsT"], w=["gmp"])
        self.tt("dve", gmp[:, :, :], gmp[:, :, :], ggu[:, :, :], ALU.mult, r=["gmp", ("ggu", 0), ("ggu", 1)], w=["gmp"])
        self.memset("dve", ssq[:, 0:2], 0.0, w=["ssq0"])
        for i in range(2):
            self.act(ggv[:, i, :], gmp[:, i, :], AF.Square, accum=ssq[:, i:i + 1], r=["gmp", "ssq0"], w=[("ggv", i), "ssq0"])
        self.rstd_from(ssq[:, 12:14], ssq[:, 0:2], 1.0 / 256.0, ["ssq0"], "ssq1")
        for i in range(2):
            self.stt("dve", mixp[:, i, :], gmp[:, i, :], ssq[:, 12 + i:13 + i], normg[:, 768:1024], ALU.mult, ALU.mult,
                     r=["gmp", "ssq1", "normg"], w=[("mixp", p, "gm", i)])

    def conv_chunk(self, j, ps, pk, raw, halo, cw, cb, cacc, xsT3, xs_tok, BT, CT, B_tok, deferred, p):
        self.act(raw[:, 3:259], ps, AF.Copy, r=[pk], w=[("raw", j % 2)])
        self.cp("pool", raw[:, 0:3], halo[:, j, :], r=[("halo", j)], w=[("rawh", j % 2)])
        self.ts("dve", cacc[:, :], raw[:, 3:259], cw[:, j, 3:4], cb[:, j:j + 1], ALU.mult, ALU.add, r=[("raw", j % 2), "cw", "cb"], w=[("cacc", j % 2)])
        for i in range(3):
            self.stt("dve", cacc[:, :], raw[:, i:i + 256], cw[:, j, i:i + 1], cacc[:, :], ALU.mult, ALU.add,
                     r=[("raw", j % 2), ("rawh", j % 2), "cw", ("cacc", j % 2)], w=[("cacc", j % 2)])
        self.cp("pool", halo[:, j, :], raw[:, 256:259], r=[("raw", j % 2)], w=[("halo", j)])
        if j < 3:
            xk = "xsT%d" % j
            self.act(xsT3[:, j, :], cacc[:, :], AF.Silu, r=[("cacc", j % 2)], w=[xk])

            def _tr(j=j, xk=xk):
                bank = 3
                tk = ("ps", bank)
                tv = self.psum[bank][:, 0:256].rearrange("p (i n) -> p i n", i=2)
                for i in range(2):
                    self.tr(tv[:, i, :], xsT3[:, j, i * 128:(i + 1) * 128], self.ident_f, r=[xk, "cstf"], w=[tk])
                self.cp("dve", xs_tok[:, :, j * 128:(j + 1) * 128], tv, r=[tk], w=[("xs", p, 0), ("xs", p, 1)])
            deferred.append(_tr)
        elif j < 5:
            g = j - 3
            self.act(BT[:, g, :], cacc[:, :], AF.Silu, r=[("cacc", j % 2)], w=[("BT", p, g)])

            def _tr(g=g):
                bank = 3
                tk = ("ps", bank)
                tv = self.psum[bank][:, 0:128].bitcast(BF16).rearrange("p (i n) -> p i n", i=2)
                for i in range(2):
                    self.tr(tv[:, i, :], BT[:, g, i * 128:(i + 1) * 128], self.ident_b, r=[("BT", p, g), "cstb0"], w=[tk])
                self.cp("dve", B_tok[:, :, g, :], tv, r=[tk], w=[("Btok", p)])
            deferred.append(_tr)
        else:
            g = j - 5
            self.act(CT[:, g, :], cacc[:, :], AF.Silu, r=[("cacc", j % 2)], w=[("CT", p, g)])

    def ssd_tile(self, l, i, t, c, p, B):
        S = self.S
        PS = self.psum
        sm, sp6, dtc, xs_tok = B["sm"], B["sp6"], B["dtc"], B["xs_tok"]
        BT, CT, B_tok, a_bc, seg, MT = B["BT"], B["CT"], B["B_tok"], B["a_bc"], B["seg"], B["MT"]
        xdt, xdtd, yo, yy, ytmp, H, Hb, sz = B["xdt"], B["xdtd"], B["yo"], B["yy"], B["ytmp"], B["H"], B["Hb"], B["sz"]
        normg, mixp, ssq = B["normg"], B["mixp"], B["ssq"]
        tc = slice(i * 128, (i + 1) * 128)
        dt = dtc[:, i, :]
        a = sm[:, 24:30]
        acol = sm[:, 30:36]
        ea = sm[:, 36:42]
        dec = sm[:, 42:48]
        cdec = sm[:, 48:54]
        xs = xs_tok[:, i, :]
        xs3 = xs.rearrange("p (h d) -> p h d", h=6)

        def b3(v):
            return v.unsqueeze(2).to_broadcast([128, 6, 64])

        def v3(ap):
            return ap.rearrange("p (h d) -> p h d", h=6)

        self.tt("dve", a, dt, sp6[:, 3, :], ALU.mult, r=[("dtc", p), "Aneg"], w=["ssd_a"])
        k5 = ("ps", 4)
        self.mm(PS[4][:, 32:38], self.U_f, a, r=["cstf", "ssd_a"], w=[k5])
        self.mm(PS[4][:, 40:46], self.ones_f, a, r=["cstf", "ssd_a"], w=[k5])
        self.cp("dve", acol, PS[4][:, 32:38], r=[k5], w=["acol"])
        self.act(ea, acol, AF.Exp, r=["acol"], w=["ea"])
        self.tt("dve", dec, PS[4][:, 40:46], acol, ALU.subtract, r=[k5, "acol"], w=["dec"])
        self.act(dec, dec, AF.Exp, r=["dec"], w=["dec"])
        self.act(cdec, PS[4][:, 40:46], AF.Exp, r=[k5], w=["cdec"])
        self.tt("dve", v3(ytmp[:, :]), xs3, b3(dt), ALU.mult, r=[("xs", p, i), ("dtc", p)], w=["ytmp"])
        self.cp("act", xdt[:, :], ytmp[:, :], r=["ytmp"], w=["xdt"])
        self.tt("dve", v3(xdtd[:, :]), v3(ytmp[:, :]), b3(dec), ALU.mult, r=["ytmp", "dec"], w=["xdtd"])
        k6, k7, k2 = ("ps", 4), ("ps", 5), ("ps", 5)
        k0l, k1l = self.pkeys(6, 0, 384), self.pkeys(7, 0, 384)
        for gi in range(2):
            self.mm(PS[4][:, 256 + gi * 128:256 + (gi + 1) * 128], BT[:, gi, tc], CT[:, gi, tc], r=[("BT", p, gi), ("CT", p, gi)], w=[k6])
        for gi in range(2):
            hs = range(3 * gi, 3 * gi + 3)
            self.cp("dve", a_bc[:, :, :], a[:, 3 * gi:3 * gi + 3].unsqueeze(2).to_broadcast([128, 3, 128]), r=["ssd_a"], w=["a_bc"])
            p7 = PS[5][:, 0:384].rearrange("p (h n) -> p h n", h=3)
            for hl in range(3):
                self.mm(p7[:, hl, :], a_bc[:, hl, :], self.U_f, r=["a_bc", "cstf"], w=[k7])
            for hl, h in enumerate(hs):
                self.stt("dve", seg[:, hl, :], p7[:, hl, :], acol[:, h:h + 1], self.negm_f, ALU.subtract, ALU.add,
                         r=[k7, "acol", "cstf"], w=["seg"])
            self.act(seg[:, :, :], seg[:, :, :], AF.Exp, r=["seg"], w=["seg"])
            self.tt("dve", MT[:, :, :], seg[:, :, :], PS[4][:, 256 + gi * 128:256 + (gi + 1) * 128].unsqueeze(1).to_broadcast([128, 3, 128]),
                    ALU.mult, r=["seg", k6], w=["MT"])
            for hl, h in enumerate(hs):
                self.mm(PS[6][:, h * 64:(h + 1) * 64], MT[:, hl, :], xdt[:, h * 64:(h + 1) * 64], r=["MT", "xdt"], w=k0l)
        for gi in range(2):
            gc = slice(gi * 192, (gi + 1) * 192)
            self.mm(PS[7][:, gc], CT[:, gi, tc], Hb[:, gc], r=[("CT", p, gi), "Hb"], w=k1l)
            self.mm(PS[5][:, gc], B_tok[:, i, gi, :], xdtd[:, gc], r=[("Btok", p), "xdtd"], w=[k2])
        self.tt("dve", v3(yo[:, :]), v3(PS[7][:, 0:384]), b3(ea), ALU.mult, r=k1l + ["ea"], w=["yo"])
        self.tt("dve", yy[:, :], PS[6][:, 0:384], yo[:, :], ALU.add, r=k0l + ["yo"], w=["yy"])
        self.tt("dve", v3(ytmp[:, :]), xs3, b3(sp6[:, 2, :]), ALU.mult, r=[("xs", p, i), "dsk"], w=["ytmp"])
        self.tt("dve", yy[:, :], yy[:, :], ytmp[:, :], ALU.add, r=["yy", "ytmp"], w=["yy"])
        self.tt("dve", yy[:, :], yy[:, :], sz[:, i, :], ALU.mult, r=["yy", ("sz", p, i)], w=["yy"])
        self.memset("dve", ssq[:, 2:4], 0.0, w=["ssq2"])
        for gi in range(2):
            gc = slice(gi * 192, (gi + 1) * 192)
            self.act(yo[:, gc], yy[:, gc], AF.Square, accum=ssq[:, 2 + gi:3 + gi], r=["yy", "ssq2"], w=["yo", "ssq2"])
        self.rstd_from(ssq[:, 4:6], ssq[:, 2:4], 1.0 / 192.0, ["ssq2"], "ssq4")
        for gi in range(2):
            gc = slice(gi * 192, (gi + 1) * 192)
            self.stt("dve", mixp[:, i, 384 + gi * 192:384 + (gi + 1) * 192], yy[:, gc], ssq[:, 4 + gi:5 + gi],
                     normg[:, 384 + gi * 192:384 + (gi + 1) * 192], ALU.mult, ALU.mult,
                     r=["yy", "ssq4", "normg"], w=[("mixp", "ssd", i)])
        self.tt("dve", v3(ytmp[:, :]), v3(H[:, :]), b3(cdec), ALU.mult, r=["H", "cdec"], w=["ytmp"])
        self.tt("dve", H[:, :], ytmp[:, :], PS[5][:, 0:384], ALU.add, r=["ytmp", k2], w=["H"])
        self.cp("act", Hb[:, :], H[:, :], r=["H"], w=["Hb"])

    def ssd_chunk(self, l, c, p, B):
        S = self.S
        PS = self.psum
        sm, sp6, dtc, xs_tok = B["sm"], B["sp6"], B["dtc"], B["xs_tok"]
        BT, CT, B_tok = B["BT"], B["CT"], B["B_tok"]
        a_bc, seg, MT, xdt, xdtd, ytmp2 = B["a_bc2"], B["seg2"], B["MT2"], B["xdt2"], B["xdtd2"], B["ytmp2"]
        yo, yy, H, Hb, sz = B["yo"], B["yy"], B["H"], B["Hb"], B["sz"]
        normg, mixp, ssq = B["normg"], B["mixp"], B["ssq"]
        a2 = sm[:, 64:76]
        acol = sm[:, 76:88]
        ea = sm[:, 88:100]
        dec = sm[:, 100:112]
        cdec = sm[:, 112:124]
        t2 = lambda v: v.rearrange("p (i h) -> p i h", i=2)
        k4, k0 = ("ps", 4), ("ps", 0)

        def v4(ap):
            return ap.rearrange("p i (h d) -> p i h d", h=6)

        def b4(v):
            return v.unsqueeze(3).to_broadcast([128, 2, 6, 64])

        def v3(ap):
            return ap.rearrange("p (h d) -> p h d", h=6)

        def b3(v):
            return v.unsqueeze(2).to_broadcast([128, 6, 64])

        self.tt("dve", t2(a2), dtc[:, :, :], sp6[:, 3:4, :].to_broadcast([128, 2, 6]), ALU.mult, r=[("dtc", p), "Aneg"], w=["ssd_a"])
        self.mm(PS[4][:, 32:44], self.U_f, a2, r=["cstf", "ssd_a"], w=[k4])
        self.mm(PS[4][:, 48:60], self.ones_f, a2, r=["cstf", "ssd_a"], w=[k4])
        self.cp("dve", acol, PS[4][:, 32:44], r=[k4], w=["acol"])
        self.act(ea, acol, AF.Exp, r=["acol"], w=["ea"])
        self.tt("dve", dec, PS[4][:, 48:60], acol, ALU.subtract, r=[k4, "acol"], w=["dec"])
        self.act(dec, dec, AF.Exp, r=["dec"], w=["dec"])
        self.act(cdec, PS[4][:, 48:60], AF.Exp, r=[k4], w=["cdec"])
        self.tt("dve", v4(ytmp2[:, :, :]), v4(xs_tok[:, :, :]), b4(dtc[:, :, :]), ALU.mult,
                r=[("xs", p, 0), ("xs", p, 1), ("dtc", p)], w=["ytmp2"])
        self.cp("act", xdt[:, :, :], ytmp2[:, :, :], r=["ytmp2"], w=["xdt"])
        self.tt("dve", v4(xdtd[:, :, :]), v4(ytmp2[:, :, :]), b4(t2(dec)), ALU.mult, r=["ytmp2", "dec"], w=["xdtd"])
        cb4 = PS[0][:, 0:512].rearrange("p (i g n) -> p i g n", i=2, g=2)
        for i in range(2):
            tc = slice(i * 128, (i + 1) * 128)
            for gi in range(2):
                self.mm(cb4[:, i, gi, :], BT[:, gi, tc], CT[:, gi, tc], r=[("BT", p, gi), ("CT", p, gi)], w=[k0])
        rb = [PS[5][:, 0:384].rearrange("p (h n) -> p h n", h=3), PS[1][:, 0:384].rearrange("p (h n) -> p h n", h=3)]
        rk = [("ps", 5), ("ps", 1)]
        yk = [("ps", 6), ("ps", 7)]
        for gi in range(2):
            self.cp("dve", a_bc[:, :, :, :], t2(a2)[:, :, 3 * gi:3 * gi + 3].unsqueeze(3).to_broadcast([128, 2, 3, 128]),
                    r=["ssd_a"], w=["a_bc"])
            for i in range(2):
                for hl in range(3):
                    self.mm(rb[i][:, hl, :], a_bc[:, i, hl, :], self.U_f, r=["a_bc", "cstf"], w=[rk[i]])
            for i in range(2):
                for hl in range(3):
                    h = 3 * gi + hl
                    self.stt("dve", seg[:, i, hl, :], rb[i][:, hl, :], acol[:, i * 6 + h:i * 6 + h + 1], self.negm_f,
                             ALU.subtract, ALU.add, r=[rk[i], "acol", "cstf"], w=["seg"])
            self.act(seg[:, :, :, :], seg[:, :, :, :], AF.Exp, r=["seg"], w=["seg"])
            self.tt("dve", MT[:, :, :, :], seg[:, :, :, :], cb4[:, :, gi, :].unsqueeze(2).to_broadcast([128, 2, 3, 128]),
                    ALU.mult, r=["seg", k0], w=["MT"])
            for i in range(2):
                for hl in range(3):
                    h = 3 * gi + hl
                    self.mm(PS[6 + i][:, h * 64:(h + 1) * 64], MT[:, i, hl, :], xdt[:, i, h * 64:(h + 1) * 64],
                            r=["MT", "xdt"], w=[yk[i]])
        k2, k3 = ("ps", 2), ("ps", 3)
        for i in range(2):
            tc = slice(i * 128, (i + 1) * 128)
            xs3 = v3(xs_tok[:, i, :])
            for gi in range(2):
                gc = slice(gi * 192, (gi + 1) * 192)
                self.mm(PS[2][:, gc], CT[:, gi, tc], Hb[:, gc], r=[("CT", p, gi), "Hb"], w=[k2])
                self.mm(PS[3][:, gc], B_tok[:, i, gi, :], xdtd[:, i, gc], r=[("Btok", p), "xdtd"], w=[k3])
            self.tt("dve", v3(yo[:, :]), v3(H[:, :]), b3(cdec[:, i * 6:i * 6 + 6]), ALU.mult, r=["H", "cdec"], w=["yo"])
            self.tt("dve", H[:, :], yo[:, :], PS[3][:, 0:384], ALU.add, r=["yo", k3], w=["H"])
            self.tt("dve", v3(yy[:, :]), v3(PS[2][:, 0:384]), b3(ea[:, i * 6:i * 6 + 6]), ALU.mult, r=[k2, "ea"], w=["yy"])
            self.cp("act", Hb[:, :], H[:, :], r=["H", k2], w=["Hb"])
            self.tt("dve", yy[:, :], yy[:, :], PS[6 + i][:, 0:384], ALU.add, r=["yy", yk[i]], w=["yy"])
            self.tt("dve", v3(yo[:, :]), xs3, b3(sp6[:, 2, :]), ALU.mult, r=[("xs", p, i), "dsk"], w=["yo"])
            self.tt("dve", yy[:, :], yy[:, :], yo[:, :], ALU.add, r=["yy", "yo"], w=["yy"])
            self.tt("dve", yy[:, :], yy[:, :], sz[:, i, :], ALU.mult, r=["yy", ("sz", p, i)], w=["yy"])
            self.memset("dve", ssq[:, 2:4], 0.0, w=["ssq2"])
            for gi in range(2):
                gc = slice(gi * 192, (gi + 1) * 192)
                self.act(yo[:, gc], yy[:, gc], AF.Square, accum=ssq[:, 2 + gi:3 + gi], r=["yy", "ssq2"], w=["yo", "ssq2"])
            self.rstd_from(ssq[:, 4:6], ssq[:, 2:4], 1.0 / 192.0, ["ssq2"], "ssq4")
            for gi in range(2):
                gc = slice(gi * 192, (gi + 1) * 192)
                self.stt("dve", mixp[:, i, 384 + gi * 192:384 + (gi + 1) * 192], yy[:, gc], ssq[:, 4 + gi:5 + gi],
                         normg[:, 384 + gi * 192:384 + (gi + 1) * 192], ALU.mult, ALU.mult,
                         r=["yy", "ssq4", "normg"], w=[("mixp", "ssd", i)])

    def attention_chunk(self, l, c, p, B):
        S = self.S
        PS = self.psum
        kT, qT, Vaug, ksum_b = B["kT"], B["qT"], B["Vaug"], B["ksum_b"]
        st12b, negM, ET = B["st12b"], B["negM"], B["ET"]
        gtmp, gtop, att_pre, rden, ssq, normg, mixp, sqt = (B["gtmp"], B["gtop"], B["att_pre"], B["rden"],
                                                                  B["ssq"], B["normg"], B["mixp"], B["sqt"])
        k5 = ("ps", 4)
        self.cp("dve", st12b[:, 0:6], B["qn2max"][:, :], r=[("qn2max", p)], w=["st12b"])
        self.cp("dve", st12b[:, 6:12], B["kn2max"][:, :], r=["kn2max"], w=["st12b"])
        self.tr(PS[4][0:12, 128:256], st12b[:, 0:12], self.ident_f, r=["st12b", "cstf"], w=[k5])
        S.add("dve", lambda e: e.reduce_max(out=negM[0:12, 7:8], in_=PS[4][0:12, 128:256], axis=AX.X), r=[k5], w=["redc"])
        self.ts("dve", B["dgt"][0:12, 0:12], self.ident_f[0:12, 0:12], negM[0:12, 7:8], None, ALU.mult, r=["redc", "cstf"], w=["dg"])
        self.mm(PS[4][:, 256:268], self.ones_f[0:12, :], B["dgt"][0:12, 0:12], r=["dg", "cstf"], w=[k5])
        self.cp("dve", st12b[:, 0:12], PS[4][:, 256:268], r=[k5], w=["st12b"])
        self.tt("dve", negM[:, 0:6], st12b[:, 0:6], st12b[:, 6:12], ALU.mult, r=["st12b"], w=["negM"])
        self.tt("pool", negM[:, 0:6], negM[:, 0:6], self.cstf[:, 713:714].to_broadcast([128, 6]), ALU.pow, r=["negM", "cstf"], w=["negM"])
        self.ts("dve", negM[:, 0:6], negM[:, 0:6], -1.0, None, ALU.mult, r=["negM"], w=["negM"])
        use_sel = c >= 4
        gsel = B["gsel"]
        if use_sel:
            for i in range(2):
                g5 = PS[4][:, 0:48].rearrange("p (h n) -> p h n", h=6)
                for h in range(6):
                    rows = slice((h % 2) * 64, (h % 2) * 64 + 64)
                    self.mm(g5[:, h, :], qT[rows, h // 2, i * 128:(i + 1) * 128], ksum_b[rows, h // 2, :],
                            r=[("qT", p, i), "ksum_b"], w=[k5])
                self.tt("dve", gtmp[:, :, :], g5, self.pastneg[:, c:c + 1, :].to_broadcast([128, 6, 8]), ALU.add,
                        r=[k5, "cstf"], w=["gtmp"])
                for h in range(6):
                    S.add("dve", (lambda h: lambda e: e.max(out=gtop[:, h, :], in_=gtmp[:, h, :]))(h), r=["gtmp"], w=["gtop"])
                self.tt("dve", gsel[:, i, :, :], gtmp[:, :, :], gtop[:, :, 2:3].to_broadcast([128, 6, 8]), ALU.is_ge,
                        r=["gtmp", "gtop"], w=["gsel"])
        nst = getattr(self, "_nst", 0)
        acc = B["acc"]
        atmp = B["atmp"]

        def s_and_exp(h, kt, q0):
            nonlocal nst
            rows = slice((h % 2) * 64, (h % 2) * 64 + 64)
            N = 256 - q0
            sb_ = 4 + nst % 2
            sk = ("ps", sb_)
            stp = PS[sb_][:, 0:N]
            self.mm(stp, kT[rows, h // 2, kt * 128:(kt + 1) * 128], qT[rows, h // 2, q0:256], start=True, stop=True,
                    r=[("kT", kt), ("qT", p, 0), ("qT", p, 1)], w=[sk])
            et = ET[nst % len(ET)]
            ek = ("ET", nst % len(ET))
            nst += 1
            self.act(et[:, 0:N], stp, AF.Exp, bias=negM[:, h:h + 1], r=[sk, "negM"], w=[ek])
            if kt >= 2 * c:
                self.tt("dve", et[:, 0:128], et[:, 0:128], self.tri_b, ALU.mult, r=[ek, "cstb1"], w=[ek])
            return et, ek

        nob = getattr(self, "_nob", 0)
        steps = []
        for h in range(6):
            if use_sel:
                kgroups = [([2 * c, 2 * c + 1], None)] + [([2 * n, 2 * n + 1], n) for n in range(c)]
            else:
                kgroups = [(list(range(2 * c + 2)), None)]
            for gi_, (kts, nsel) in enumerate(kgroups):
                ob = nob % 2
                nob += 1
                for kt in kts:
                    steps.append(dict(h=h, kt=kt, ob=ob, first=(kt == kts[0]), lastk=(kt == kts[-1]), nsel=nsel,
                                      lastg=(gi_ == len(kgroups) - 1)))

        def pv_and_post(st, et, ek):
            h, kt, ob = st["h"], st["kt"], st["ob"]
            okl = [("ps", 6), ("ps", 7)] if ob == 0 else [("ps", 2), ("ps", 3)]
            O = (self.ps67 if ob == 0 else self.ps23)[:, :].rearrange("p (b x) -> p b x", b=2)[:, :, 0:65]
            if kt <= 2 * c:
                q0, qtiles = 0, [0, 1]
            else:
                q0, qtiles = 128, [1]
            first = st["first"]
            for qi in qtiles:
                e0 = qi * 128 - q0
                self.mm(O[:, qi, :], et[:, e0:e0 + 128], Vaug[:, kt, h * 65:(h + 1) * 65], start=first,
                        stop=(st["lastk"] or (kt == 2 * c and qi == 0)), r=[ek, ("V", kt)], w=[okl[qi]])
            if not st["lastk"]:
                return
            nsel = st["nsel"]
            if not use_sel:
                src = O
                sk_ = list(okl)
            elif nsel is None:
                self.cp("dve", acc[:, :, :], O, r=okl, w=["acc"])
                return
            else:
                selb = gsel[:, :, h, nsel].unsqueeze(2).to_broadcast([128, 2, 65])
                self.tt("dve", atmp[:, :, :], O, selb, ALU.mult, r=okl + ["gsel"], w=["atmp"])
                self.tt("dve", acc[:, :, :], acc[:, :, :], atmp[:, :, :], ALU.add, r=["acc", "atmp"], w=["acc"])
                if not st["lastg"]:
                    return
                src = acc
                sk_ = ["acc"]
            S.add("dve", (lambda src: lambda e: e.reciprocal(out=rden[:, 0:2], in_=src[:, :, 64]))(src), r=sk_, w=["rden"])
            self.tt("dve", att_pre[:, :, h * 64:(h + 1) * 64], src[:, :, 0:64],
                    rden[:, 0:2].unsqueeze(2).to_broadcast([128, 2, 64]), ALU.mult, r=sk_ + ["rden"], w=["attpre"])

        LOOK = 2
        pend = []
        for st in steps:
            q0 = 0 if st["kt"] <= 2 * c else 128
            et, ek = s_and_exp(st["h"], st["kt"], q0)
            pend.append((st, et, ek))
            if len(pend) > LOOK:
                pv_and_post(*pend.pop(0))
            yield
        while pend:
            pv_and_post(*pend.pop(0))
        yield
        self._nob = nob
        self._nst = nst
        self.memset("dve", ssq[:, 6:8], 0.0, w=["ssq6"])
        for qi in range(2):
            self.act(sqt[:, :], att_pre[:, qi, :], AF.Square, accum=ssq[:, 6 + qi:7 + qi], r=["attpre", "ssq6"], w=["yo", "ssq6"])
        self.rstd_from(rden[:, 2:4], ssq[:, 6:8], 1.0 / 384.0, ["ssq6"], "rden2")
        for qi in range(2):
            self.stt("dve", mixp[:, qi, 0:384], att_pre[:, qi, :], rden[:, 2 + qi:3 + qi], normg[:, 0:384], ALU.mult, ALU.mult,
                     r=["attpre", "rden2", "normg"], w=[("mixp", "att", qi)])

    def ffn_layout(self):
        self.arena_reset()
        A = self.aalloc
        self.PB = A([128, 2, D], F32)
        self.hb2 = [A([128, D], BF16) for _ in range(2)]
        self.wp = [A([128, 4096], BF16) for _ in range(6)]
        self.actT = [A([128, 4, 512], BF16) for _ in range(2)]
        self.sg = [A([128, 512], F32) for _ in range(2)]
        self.gates = A([128, NT, 8], F32)
        self.Wr = A([128, 8, 8], BF16)
        self.rt_lg = A([128, 8], F32)
        self.rt_top = A([128, 8], F32)
        self.rt_p = A([128, 4], F32)
        self.rt_g = A([128, 2, 8], F32)
        self.slice_n = 0

    def wout_ln1(self, l):
        self.ffn_layout()
        PS = self.psum
        self.load_ln_params(self.ln1_g[l, :], self.ln1_b[l, :])
        for half in range(2):
            self.dma(self.wp[half][:, :].rearrange("p (kc n) -> p kc n", kc=8),
                     self.w_out[l, :, half * 512:(half + 1) * 512].rearrange("(kc p) n -> p kc n", p=128),
                     w=[("wp", half)], eng="pool", lane="wp%d" % half)
        banks = [0, 1, 2, 5]
        pend = None
        for t in range(NT):
            for half in range(2):
                bk = banks[(2 * t + half) % 4]
                pk = ("ps", bk)
                ws = self.wp[half][:, :].rearrange("p (kc n) -> p kc n", kc=8)
                for kc in range(8):
                    self.mm(PS[bk][:, 0:512], self.XT[:, kc, t * 128:(t + 1) * 128], ws[:, kc, :], start=(kc == 0), stop=(kc == 7),
                            r=[("XT", t, kc), ("wp", half)], w=[pk])
                hs = self.h_acc[:, t, half * 512:(half + 1) * 512]
                self.tt("dve", hs, hs, PS[bk][:, 0:512], ALU.add, r=[("h", t), pk], w=[("h", t)])
            pn = self.ln_tile(t, final=False, defer=True)
            if pend is not None:
                pend()
            pend = pn
        if pend is not None:
            pend()

    def ffn_core(self, wg, wu, wd, dff, gate_of):
        PS = self.psum
        nsl = (dff + 511) // 512
        for s_ in range(nsl):
            f0 = s_ * 512
            fw = min(512, dff - f0)
            nfc = fw // 128
            base = (self.slice_n % 2) * 3
            self.slice_n += 1
            sg_, su_, sd_ = base, base + 1, base + 2
            wgv = self.wp[sg_][:, 0:8 * fw].rearrange("p (kc n) -> p kc n", kc=8)
            wuv = self.wp[su_][:, 0:8 * fw].rearrange("p (kc n) -> p kc n", kc=8)
            wdv = self.wp[sd_][:, 0:nfc * 1024].rearrange("p (fc n) -> p fc n", fc=nfc)
            self.dma(wgv, wg[:, f0:f0 + fw].rearrange("(kc p) n -> p kc n", p=128), w=[("wp", sg_)], eng="pool", lane="wp%d" % sg_)
            self.dma(wuv, wu[:, f0:f0 + fw].rearrange("(kc p) n -> p kc n", p=128), w=[("wp", su_)], eng="pool", lane="wp%d" % su_)
            self.dma(wdv, wd[f0:f0 + fw, :].rearrange("(fc p) n -> p fc n", p=128), w=[("wp", sd_)], eng="pool", lane="wp%d" % sd_)
            for tcx in range(4):
                ab = self.ab_n % 2
                self.ab_n += 1
                aT = self.actT[ab]
                cols = slice(tcx * 512, (tcx + 1) * 512)
                xk = lambda kc: [("XT", t, kc) for t in range(4 * tcx, 4 * tcx + 4)]
                for fc in range(nfc):
                    gb, ub = fc % 2, 2 + fc % 2
                    for kc in range(8):
                        self.mm(PS[gb][:, 0:512], wgv[:, kc, fc * 128:(fc + 1) * 128], self.XT[:, kc, cols], start=(kc == 0),
                                stop=(kc == 7), r=xk(kc) + [("wp", sg_)], w=[("ps", gb)])
                    for kc in range(8):
                        self.mm(PS[ub][:, 0:512], wuv[:, kc, fc * 128:(fc + 1) * 128], self.XT[:, kc, cols], start=(kc == 0),
                                stop=(kc == 7), r=xk(kc) + [("wp", su_)], w=[("ps", ub)])
                    sgb = self.sg[fc % 2]
                    self.act(sgb[:, :], PS[gb][:, 0:512], AF.Silu, r=[("ps", gb)], w=[("sg", fc % 2)])
                    self.tt("dve", aT[:, fc, :], sgb[:, :], PS[ub][:, 0:512], ALU.mult, r=[("sg", fc % 2), ("ps", ub)], w=[("actT", ab)])
                def down_fn(tcx=tcx, aT=aT, ab=ab, wdv=wdv, sd_=sd_, nfc=nfc):
                    for i in range(4):
                        t = 4 * tcx + i
                        for half in range(2):
                            db = 4 + (2 * i + half) % 4
                            for fc in range(nfc):
                                self.mm(PS[db][:, 0:512], aT[:, fc, i * 128:(i + 1) * 128], wdv[:, fc, half * 512:(half + 1) * 512],
                                        start=(fc == 0), stop=(fc == nfc - 1), r=[("actT", ab), ("wp", sd_)], w=self.pkeys(db))
                            hs = self.h_acc[:, t, half * 512:(half + 1) * 512]
                            g = gate_of(t)
                            if g is None:
                                self.tt("dve", hs, hs, PS[db][:, 0:512], ALU.add, r=[("h", t)] + self.pkeys(db), w=[("h", t)])
                            else:
                                self.stt("dve", hs, PS[db][:, 0:512], g, hs, ALU.mult, ALU.add, r=[("h", t), "gates"] + self.pkeys(db), w=[("h", t)])
                if self.pend_down is not None:
                    self.pend_down()
                self.pend_down = down_fn

    def flush_down(self):
        if getattr(self, "pend_down", None) is not None:
            self.pend_down()
        self.pend_down = None

    def ffn(self, l):
        self.ab_n = 0
        self.pend_down = None
        self.ffn_core(self.ffn_wg[l // 2, :, :], self.ffn_wu[l // 2, :, :], self.ffn_wd[l // 2, :, :], D_FF, lambda t: None)
        self.flush_down()

    def moe(self, l):
        S = self.S
        PS = self.psum
        self.ab_n = 0
        self.pend_down = None
        m = l // 2
        self.dma(self.Wr[:, :, :], self.router_w[m, :, :].rearrange("(kc p) e -> p kc e", p=128), w=["Wr"], eng="pool", lane="wp5")
        lg, top, p, g2 = self.rt_lg, self.rt_top, self.rt_p, self.rt_g
        for t in range(NT):
            pk = ("ps", 5)
            for kc in range(8):
                self.mm(PS[5][:, 0:8], self.XT[:, kc, t * 128:(t + 1) * 128], self.Wr[:, kc, :], start=(kc == 0), stop=(kc == 7),
                        r=[("XT", t, kc), "Wr"], w=[pk])
            self.cp("dve", lg[:, :], PS[5][:, 0:8], r=[pk], w=["rt_lg"])
            S.add("dve", lambda e: e.max(out=top[:, :], in_=lg[:, :]), r=["rt_lg"], w=["rt_top"])
            self.tt("dve", p[:, 0:1], top[:, 1:2], top[:, 0:1], ALU.subtract, r=["rt_top"], w=["rt_p"])
            self.act(p[:, 1:2], p[:, 0:1], AF.Sigmoid, r=["rt_p"], w=["rt_p2"])
            self.ts("dve", p[:, 2:3], p[:, 1:2], -1.0, 1.0, ALU.mult, ALU.add, r=["rt_p2"], w=["rt_p1"])
            self.ts("dve", g2[:, 0, :], lg[:, :], top[:, 0:1], p[:, 2:3], ALU.is_equal, ALU.mult, r=["rt_lg", "rt_top", "rt_p1"], w=["rt_g0"])
            self.ts("dve", g2[:, 1, :], lg[:, :], top[:, 1:2], p[:, 1:2], ALU.is_equal, ALU.mult, r=["rt_lg", "rt_top", "rt_p2"], w=["rt_g1"])
            self.tt("dve", self.gates[:, t, :], g2[:, 0, :], g2[:, 1, :], ALU.add, r=["rt_g0", "rt_g1"], w=["gates"])
        self.dump("gates", self.gates[:, :, :], r=["gates"])
        nexp = getattr(self, "n_experts", NEXP)
        for e_ in range(nexp):
            self.ffn_core(self.moe_wg[m, e_, :, :], self.moe_wu[m, e_, :, :], self.moe_wd[m, e_, :, :], D_FFE,
                          (lambda e_: lambda t: self.gates[:, t, e_:e_ + 1])(e_))
        self.flush_down()


def _consts():
    c = np.zeros((128, 848), np.float32)
    i = np.arange(128)
    c[:, 0:128] = np.eye(128, dtype=np.float32)
    c[:, 128:256] = (i[:, None] <= i[None, :]).astype(np.float32)
    c[:, 256:384] = np.where(i[None, :] >= i[:, None], 0.0, -BIG)
    c[:, 384:512] = (i[None, :] <= i[:, None]).astype(np.float32)
    c[:, 512:640] = 1.0
    pn = np.where(np.arange(8)[None, :] < np.arange(8)[:, None], 0.0, -1e30).astype(np.float32)
    c[:, 640:704] = pn.reshape(1, 64)
    inv = (500000.0 ** (-np.arange(0, 16, 2, dtype=np.float32) / 16.0)).astype(np.float32)
    c[:, 704:712] = inv[None, :]
    c[:, 712] = -0.5
    c[:, 713] = 0.5
    oneh = np.zeros((8, 8, 128), np.float32)
    for n in range(8):
        oneh[n, n, :] = 1.0
    return c, oneh.reshape(8, 1024)


_CACHE = {}


def _prep_shared(inputs):
    f = lambda a: np.ascontiguousarray(np.asarray(a, dtype=np.float32))
    cst, oneh = _consts()
    sh = {
        "cst": cst, "oneh": oneh,
        "ln_in_g": f(inputs["ln_in_g"]), "ln_in_b": f(inputs["ln_in_b"]),
        "w_in": f(inputs["w_in"]),
        "conv_wT": f(np.asarray(inputs["conv_w"]).reshape(DEPTH, 4, 7, 128).transpose(0, 3, 2, 1)),
        "conv_bT": f(np.asarray(inputs["conv_b"]).reshape(DEPTH, 7, 128).transpose(0, 2, 1)),
        "dt_bias": f(inputs["dt_bias"]), "a_log": f(inputs["a_log"]), "d_skip": f(inputs["d_skip"]),
        "normg": f(np.concatenate([np.asarray(inputs["att_norm_g"]), np.asarray(inputs["ssd_norm_g"]),
                                   np.asarray(inputs["gm_norm_g"])], axis=1)),
        "gm_ln_g": f(inputs["gm_ln_g"]), "gm_ln_b": f(inputs["gm_ln_b"]),
        "gm_w_s": f(inputs["gm_w_s"]),
        "gm_b_sT": f(np.asarray(inputs["gm_b_s"]).transpose(0, 2, 1)),
        "w_out": f(inputs["w_out"]),
        "ln1_g": f(inputs["ln1_g"]), "ln1_b": f(inputs["ln1_b"]),
        "ln2_g": f(inputs["ln2_g"]), "ln2_b": f(inputs["ln2_b"]),
        "ffn_w_gate": f(inputs["ffn_w_gate"]), "ffn_w_up": f(inputs["ffn_w_up"]), "ffn_w_down": f(inputs["ffn_w_down"]),
        "router_w": f(inputs["router_w"]),
        "moe_w_gate": f(inputs["moe_w_gate"]), "moe_w_up": f(inputs["moe_w_up"]), "moe_w_down": f(inputs["moe_w_down"]),
    }
    return sh


def run(inputs, cores=8, dbg=(), stop=None, n_layers=DEPTH, trace=False):
    b = Builder(n_layers=n_layers, dbg=dbg, stop=stop)
    nc = b.build()
    sh = _prep_shared(inputs)
    x = np.asarray(inputs["x"], dtype=np.float32)
    pos = np.asarray(inputs["positions"]).astype(np.int32)
    in_maps = []
    for c in range(cores):
        m = dict(sh)
        m["x"] = np.ascontiguousarray(x[c])
        m["pos"] = np.ascontiguousarray(pos[c].reshape(NT, 128).T)
        in_maps.append(m)
    res = run_bass_kernel_spmd(nc, in_maps, core_ids=list(range(cores)), trace=trace)
    return res, b


def kernel(**inputs):
    res, b = run(inputs)
    out = np.stack([np.asarray(r["y"]) for r in res.results], axis=0)
    return out.astype(np.float32)
```

```python
import numpy as np
from contextlib import ExitStack
import concourse.bass as bass
import concourse.mybir as mybir
from concourse.bass_utils import run_bass_kernel_spmd

F32 = mybir.dt.float32
BF16 = mybir.dt.bfloat16
I32 = mybir.dt.int32
AF = mybir.ActivationFunctionType
ALU = mybir.AluOpType
AX = mybir.AxisListType

DEPTH = 2
ALPHA = (2.0 * DEPTH) ** 0.25
EPS = 1e-5
S_LEN = 2048
D = 1024
NT = 16
D_IN = 2950
D_FF = 2816
D_FFE = 3584
NEXP = 8
BIG = 30000.0


class _Op:
    __slots__ = ("eng", "fn", "deps", "isdma", "lane", "lane_cnt", "need_inc", "cnt", "tag", "iname")


class Sched:
    def __init__(self):
        self.ops = []
        self.state = {}
        self.lane_last = {}
        self.lane_n = {}
        self.fence_deps = {}
        self.tag = ""

    def add(self, eng, fn, r=(), w=(), lane=None):
        idx = len(self.ops)
        op = _Op()
        op.eng = eng
        op.fn = fn
        op.isdma = lane is not None
        op.lane = lane
        op.need_inc = False
        op.cnt = 0
        op.tag = self.tag
        op.iname = None
        deps = dict(self.fence_deps)
        for k in r:
            st = self.state.get(k)
            if st is not None and st[0] is not None:
                deps[st[0]] = True
        for k in w:
            st = self.state.get(k)
            if st is not None:
                if st[0] is not None:
                    deps.setdefault(st[0], False)
                for ri in st[1].values():
                    deps.setdefault(ri, False)
                for ri in st[2]:
                    deps.setdefault(ri, False)
        if lane is not None:
            prev = self.lane_last.get(lane)
            if prev is not None:
                deps[prev] = True
            self.lane_last[lane] = idx
            self.lane_n[lane] = self.lane_n.get(lane, 0) + 1
            op.lane_cnt = 16 * self.lane_n[lane]
        for k in r:
            st = self.state.setdefault(k, [None, {}, []])
            if lane is not None:
                st[2].append(idx)
            else:
                st[1][eng] = idx
        for k in w:
            self.state[k] = [idx, {}, []]
        deps.pop(idx, None)
        op.deps = deps
        self.ops.append(op)
        return idx

    def fence(self):
        last = {}
        for i, op in enumerate(self.ops):
            if op.isdma:
                last[("lane", op.lane)] = i
            else:
                last[op.eng] = i
        self.fence_deps = {i: True for i in last.values()}

    def emit(self, nc, stack):
        ops = self.ops
        for op in ops:
            for d, strong in op.deps.items():
                dop = ops[d]
                if dop.isdma:
                    continue
                if dop.eng == op.eng and not op.isdma:
                    if op.eng == "pe":
                        continue
                dop.need_inc = True
        cnt = {}
        for op in ops:
            if not op.isdma and op.need_inc:
                cnt[op.eng] = cnt.get(op.eng, 0) + 1
                op.cnt = cnt[op.eng]
        engs = ["pe", "act", "dve", "pool", "sp"]
        sems = {e: stack.enter_context(nc.semaphore("s_" + e)) for e in engs}
        lanes = sorted(self.lane_n.keys(), key=str)
        lsems = {l: stack.enter_context(nc.semaphore("l_" + str(l))) for l in lanes}
        per = {e: [] for e in engs}
        for i, op in enumerate(ops):
            per[op.eng].append(op)
        final_lane = {l: 16 * n for l, n in self.lane_n.items()}

        def body(eng_name):
            def f(e):
                waited = {}
                for op in per[eng_name]:
                    for d in sorted(op.deps):
                        strong = op.deps[d]
                        dop = ops[d]
                        if dop.isdma:
                            sem, val = lsems[dop.lane], dop.lane_cnt
                            key = ("l", dop.lane)
                        else:
                            if dop.eng == op.eng and not op.isdma:
                                if op.eng == "pe":
                                    continue
                            sem, val = sems[dop.eng], dop.cnt
                            key = ("e", dop.eng)
                        if waited.get(key, 0) >= val:
                            continue
                        e.wait_ge(sem, val)
                        waited[key] = val
                    ins = op.fn(e)
                    try:
                        op.iname = ins.ins.name
                    except Exception:
                        pass
                    if op.isdma:
                        ins.then_inc(lsems[op.lane], 16)
                    elif op.need_inc:
                        ins.then_inc(sems[op.eng], 1)
                if eng_name == "sp":
                    for l in lanes:
                        e.wait_ge(lsems[l], final_lane[l])
            return f

        with nc.Block() as block:
            block.tensor(body("pe"))
            block.scalar(body("act"))
            block.vector(body("dve"))
            block.gpsimd(body("pool"))
            block.sync(body("sp"))


class Builder:
    def __init__(self, n_layers=DEPTH, dbg=(), stop=None):
        self.nc = bass.Bass("TRN2", target_bir_lowering=False)
        self.S = Sched()
        self.stack = ExitStack()
        self.n_layers = n_layers
        self.dbg = set(dbg)
        self.stop = stop
        self.dbg_outs = []
        self.lane_rr = 0
        self.tcount = 0

    def dram_in(self, name, shape, dt=F32):
        return self.nc.dram_tensor(name, list(shape), dt, kind="ExternalInput").ap()

    def sb(self, name, shape, dt):
        return self.stack.enter_context(self.nc.sbuf_tensor(name, list(shape), dt))

    def splane(self):
        self.lane_rr = (self.lane_rr + 1) % 8
        return "sp%d" % self.lane_rr

    def mm(self, out, lhsT, rhs, start=True, stop=True, r=(), w=()):
        self.S.add("pe", lambda e: e.matmul(out, lhsT, rhs, start=start, stop=stop), r, w)

    def tr(self, out, in_, ident, r=(), w=()):
        self.S.add("pe", lambda e: e.transpose(out, in_, ident), r, w)

    def act(self, out, in_, func, bias=None, scale=None, accum=None, r=(), w=()):
        kw = {}
        if bias is not None:
            kw["bias"] = bias
        if scale is not None:
            kw["scale"] = scale
        if accum is not None:
            kw["accum_out"] = accum
        self.S.add("act", lambda e: e.activation(out=out, in_=in_, func=func, **kw), r, w)

    def tt(self, eng, out, in0, in1, op, r=(), w=()):
        self.S.add(eng, lambda e: e.tensor_tensor(out=out, in0=in0, in1=in1, op=op), r, w)

    def ts(self, eng, out, in0, s1, s2, op0, op1=None, r=(), w=()):
        if op1 is None:
            self.S.add(eng, lambda e: e.tensor_scalar(out=out, in0=in0, scalar1=s1, scalar2=None, op0=op0), r, w)
        else:
            self.S.add(eng, lambda e: e.tensor_scalar(out=out, in0=in0, scalar1=s1, scalar2=s2, op0=op0, op1=op1), r, w)

    def stt(self, eng, out, in0, scalar, in1, op0, op1, r=(), w=()):
        self.S.add(eng, lambda e: e.scalar_tensor_tensor(out=out, in0=in0, scalar=scalar, in1=in1, op0=op0, op1=op1), r, w)

    def cp(self, eng, out, in_, r=(), w=()):
        if eng == "act":
            self.S.add("act", lambda e: e.copy(out=out, in_=in_), r, w)
        else:
            self.S.add(eng, lambda e: e.tensor_copy(out=out, in_=in_), r, w)

    def memset(self, eng, ap, val, w=()):
        self.S.add(eng, lambda e: e.memset(ap, val), (), w)

    def dma(self, out, in_, r=(), w=(), eng="sp", lane=None):
        if lane is None:
            lane = self.splane()
        self.S.add(eng, lambda e: e.dma_start(out=out, in_=in_), r, w, lane=lane)

    def dump(self, name, ap, r):
        if name not in self.dbg:
            return
        t = self.nc.dram_tensor("dbg_" + name, list(ap.shape), ap.dtype, kind="ExternalOutput").ap()
        self.dbg_outs.append("dbg_" + name)
        self.dma(t, ap, r=r, w=[("dbgout", name)])

    def pkeys(self, bank, c0=0, c1=512):
        return [("ps", bank)]

    def xtk(self, t):
        return [("XT", t, kc) for kc in range(8)]

    def arena_reset(self):
        self.a_off = 0

    def aalloc(self, shape, dt, at=None):
        n = int(np.prod(shape[1:]))
        nb = n * (2 if dt == BF16 else 4)
        nb = (nb + 3) // 4 * 4
        if at is None:
            off = self.a_off
            self.a_off += nb
        else:
            off = at
        self.last_off = off
        assert self.a_off <= self.ARENA_BYTES, (self.a_off, self.ARENA_BYTES)
        v = self.arena[0:shape[0], off // 2:(off + nb) // 2]
        if dt != BF16:
            v = v.bitcast(dt)
        v = v[:, 0:n]
        if len(shape) == 3:
            v = v.rearrange("p (a b) -> p a b", a=shape[1])
        elif len(shape) == 4:
            v = v.rearrange("p (a b c) -> p a b c", a=shape[1], b=shape[2])
        return v

    def build(self):
        nc = self.nc
        L = self.n_layers
        self.x = self.dram_in("x", [S_LEN, D])
        self.pos = self.dram_in("pos", [128, NT], I32)
        self.cst = self.dram_in("cst", [128, 848])
        self.oneh = self.dram_in("oneh", [8, 8 * 128])
        self.ln_in_g = self.dram_in("ln_in_g", [D])
        self.ln_in_b = self.dram_in("ln_in_b", [D])
        self.w_in = self.dram_in("w_in", [DEPTH, D, D_IN])
        self.conv_wT = self.dram_in("conv_wT", [DEPTH, 128, 7, 4])
        self.conv_bT = self.dram_in("conv_bT", [DEPTH, 128, 7])
        self.dt_bias = self.dram_in("dt_bias", [DEPTH, 6])
        self.a_log = self.dram_in("a_log", [DEPTH, 6])
        self.d_skip = self.dram_in("d_skip", [DEPTH, 6])
        self.normg = self.dram_in("normg", [DEPTH, D])
        self.gm_ln_g = self.dram_in("gm_ln_g", [DEPTH, 256])
        self.gm_ln_b = self.dram_in("gm_ln_b", [DEPTH, 256])
        self.gm_w_s = self.dram_in("gm_w_s", [DEPTH, 4, 128, 128])
        self.gm_b_sT = self.dram_in("gm_b_sT", [DEPTH, 128, 4])
        self.w_out = self.dram_in("w_out", [DEPTH, D, D])
        self.ln1_g = self.dram_in("ln1_g", [DEPTH, D])
        self.ln1_b = self.dram_in("ln1_b", [DEPTH, D])
        self.ln2_g = self.dram_in("ln2_g", [DEPTH, D])
        self.ln2_b = self.dram_in("ln2_b", [DEPTH, D])
        self.ffn_wg = self.dram_in("ffn_w_gate", [1, D, D_FF])
        self.ffn_wu = self.dram_in("ffn_w_up", [1, D, D_FF])
        self.ffn_wd = self.dram_in("ffn_w_down", [1, D_FF, D])
        self.router_w = self.dram_in("router_w", [1, D, NEXP])
        self.moe_wg = self.dram_in("moe_w_gate", [1, NEXP, D, D_FFE])
        self.moe_wu = self.dram_in("moe_w_up", [1, NEXP, D, D_FFE])
        self.moe_wd = self.dram_in("moe_w_down", [1, NEXP, D_FFE, D])
        self.y = nc.dram_tensor("y", [S_LEN, D], F32, kind="ExternalOutput").ap()
        self.w_in_bf = nc.dram_tensor("w_in_bf", [DEPTH, D, D_IN], BF16, kind="Internal").ap()

        self.h_acc = self.sb("h_acc", [128, NT, D], F32)
        self.XT = self.sb("XT", [128, 8, S_LEN], BF16)
        self.cstf = self.sb("cstf", [128, 848], F32)
        self.cstb = self.sb("cstb", [128, 256], BF16)
        self.lnst2 = [self.sb("lnst%d" % i, [128, 2, 6], F32) for i in range(2)]
        self.lnmv2 = [self.sb("lnmv%d" % i, [128, 4], F32) for i in range(2)]
        self.lnst, self.lnmv = self.lnst2[0], self.lnmv2[0]
        self.cs = self.sb("cs", [128, 2, NT, 8], F32)
        self.ARENA_BYTES = 109408
        self.arena = self.sb("arena", [128, self.ARENA_BYTES // 2], BF16)
        p_ = {}
        for i in (0, 1, 4, 5):
            p_[i] = self.stack.enter_context(nc.psum_tensor("ps%d" % i, [128, 512], F32))
        self.ps23 = self.stack.enter_context(nc.psum_tensor("ps23", [128, 1024], F32))
        self.ps67 = self.stack.enter_context(nc.psum_tensor("ps67", [128, 1024], F32))
        p_[2], p_[3] = self.ps23[:, 0:512], self.ps23[:, 512:1024]
        p_[6], p_[7] = self.ps67[:, 0:512], self.ps67[:, 512:1024]
        self.psum = [p_[i] for i in range(8)]

        self.ident_f = self.cstf[:, 0:128]
        self.U_f = self.cstf[:, 128:256]
        self.negm_f = self.cstf[:, 256:384]
        self.tril_f = self.cstf[:, 384:512]
        self.ones_f = self.cstf[:, 512:640]
        self.pastneg = self.cstf[:, 640:704].rearrange("p (b n) -> p b n", b=8)
        self.invf = self.cstf[:, 704:712]
        self.ident_b = self.cstb[:, 0:128]
        self.tri_b = self.cstb[:, 128:256]

        self.dma(self.cstf[:, :], self.cst[:, :], w=["cstf"])
        self.dma(self.cstb[:, 0:128], self.cst[:, 0:128], w=["cstb0"], eng="pool", lane="pw0")
        self.dma(self.cstb[:, 128:256], self.cst[:, 128:256], w=["cstb1"], eng="pool", lane="pw1")
        self.CK = ["cstf", "cstb0", "cstb1"]

        self.S.tag = "init"
        def convert_w_in(l_):
            for r_ in range(4):
                self.dma(self.w_in_bf[l_, r_ * 256:(r_ + 1) * 256, :], self.w_in[l_, r_ * 256:(r_ + 1) * 256, :],
                         w=[("w_in_bf", l_)], eng="pool", lane="cv%d" % r_)
        convert_w_in(0)
        self.rope_tables()
        self.load_ln_params(self.ln_in_g, self.ln_in_b)
        for t in range(NT):
            self.dma(self.h_acc[:, t, :], self.x[t * 128:(t + 1) * 128, :], w=[("h", t)])
        pend = None
        for t in range(NT):
            pn = self.ln_tile(t, final=False, defer=True)
            if pend is not None:
                pend()
            pend = pn
        if pend is not None:
            pend()
        self.S.fence()
        for l_ in range(1, L):
            convert_w_in(l_)
        self.dump("h0", self.h_acc[:, 0:2, :], r=[("h", 0), ("h", 1)])
        self.dump("XT0", self.XT[:, :, 0:256], r=self.xtk(0) + self.xtk(1))
        if self.stop == "ln_in":
            return self.finish()

        for l in range(L):
            self.S.tag = "mixer%d" % l
            self.mixer(l)
            if self.stop in ("proj", "ssd", "att"):
                return self.finish()
            if self.stop == "mixer%d" % l:
                return self.finish()
            self.S.fence()
            self.S.tag = "wout%d" % l
            self.wout_ln1(l)
            if self.stop == "ln1_%d" % l:
                return self.finish()
            self.S.tag = "ffn%d" % l
            if l % 2 == 0:
                self.ffn(l)
            else:
                self.moe(l)
            self.S.tag = "ln2_%d" % l
            last = (l == L - 1) and self.stop is None
            self.load_ln_params(self.ln2_g[l, :], self.ln2_b[l, :])
            pend = None
            for t in range(NT):
                pn = self.ln_tile(t, final=last, need_xt=(not last), defer=True)
                if pend is not None:
                    pend()
                pend = pn
            if pend is not None:
                pend()
            if self.stop == "ln2_%d" % l:
                return self.finish()
            self.S.fence()
        return self.finish()

    def finish(self):
        if self.stop is not None:
            for t in range(NT):
                self.dma(self.y[t * 128:(t + 1) * 128, :], self.h_acc[:, t, :], r=[("h", t)], w=[("y", t)])
        self.S.emit(self.nc, self.stack)
        self.stack.close()
        return self.nc

    def rope_tables(self):
        self.arena_reset()
        self.PB = self.aalloc([128, 2, D], F32)
        self.hb2 = [self.aalloc([128, D], BF16) for _ in range(2)]
        posi = self.aalloc([128, NT], I32)
        posf = self.aalloc([128, NT], F32)
        ang = self.aalloc([128, NT, 8], F32)
        tmp = self.aalloc([128, NT, 8], F32)
        self.dma(posi[:, :], self.pos[:, :], w=["posi"])
        self.cp("dve", posf[:, :], posi[:, :], r=["posi"], w=["posf"])
        self.tt("dve", ang[:, :, :], posf[:, :].unsqueeze(2).to_broadcast([128, NT, 8]),
                self.invf.unsqueeze(1).to_broadcast([128, NT, 8]), ALU.mult, r=["posf", "cstf"], w=["ang"])
        ki = self.aalloc([128, NT, 8], I32)
        kf = self.aalloc([128, NT, 8], F32)
        for which, shift in ((0, 0.75), (1, 0.5)):
            self.ts("dve", tmp[:, :, :], ang[:, :, :], float(1.0 / (2.0 * np.pi)), float(shift), ALU.mult, ALU.add, r=["ang"], w=["angt"])
            self.cp("dve", ki[:, :, :], tmp[:, :, :], r=["angt"], w=["angk"])
            self.cp("dve", kf[:, :, :], ki[:, :, :], r=["angk"], w=["angkf"])
            self.tt("dve", tmp[:, :, :], tmp[:, :, :], kf[:, :, :], ALU.subtract, r=["angt", "angkf"], w=["angt"])
            self.ts("dve", kf[:, :, :], tmp[:, :, :], 1.0, None, ALU.is_ge, r=["angt"], w=["angkf"])
            self.tt("dve", tmp[:, :, :], tmp[:, :, :], kf[:, :, :], ALU.subtract, r=["angt", "angkf"], w=["angt"])
            self.ts("dve", kf[:, :, :], tmp[:, :, :], 0.0, None, ALU.is_lt, r=["angt"], w=["angkf"])
            self.tt("dve", tmp[:, :, :], tmp[:, :, :], kf[:, :, :], ALU.add, r=["angt", "angkf"], w=["angt"])
            self.ts("dve", tmp[:, :, :], tmp[:, :, :], -0.5, float(2.0 * np.pi), ALU.add, ALU.mult, r=["angt"], w=["angt"])
            self.ts("dve", tmp[:, :, :], tmp[:, :, :], float(-np.pi), float(np.pi), ALU.max, ALU.min, r=["angt"], w=["angt"])
            self.act(self.cs[:, which, :, :], tmp[:, :, :], AF.Sin, r=["angt"], w=[("cs", which)])

    def load_ln_params(self, g, b):
        self.dma(self.PB[:, 0, :], g.partition_broadcast(128), w=["PBg"])
        self.dma(self.PB[:, 1, :], b.partition_broadcast(128), w=["PBb"])

    def ln_tile(self, t, final, need_xt=True, defer=False):
        src = self.h_acc[:, t, :]
        hk = ("h", t)
        q_ = t % 2
        st, mv, hb = self.lnst2[q_], self.lnmv2[q_], self.hb2[q_]
        kst, kmv, krs, knm, khb = ("lnst", q_), ("lnmv", q_), ("lnrs", q_), ("lnnm", q_), ("hb", q_)
        for c in range(2):
            self.S.add("dve", (lambda c: lambda e: e.bn_stats(out=st[:, c, :], in_=src[:, c * 512:(c + 1) * 512]))(c),
                       r=[hk], w=[(kst, c)])
        self.S.add("dve", lambda e: e.bn_aggr(out=mv[:, 0:2], in_=st[:, :, :]), r=[(kst, 0), (kst, 1)], w=[kmv])
        self.ts("dve", mv[:, 2:3], mv[:, 1:2], EPS, None, ALU.add, r=[kmv], w=[krs])
        self.tt("pool", mv[:, 2:3], mv[:, 2:3], self.cstf[:, 712:713], ALU.pow, r=[krs, "cstf"], w=[krs])
        self.stt("dve", mv[:, 3:4], mv[:, 0:1], -1.0, mv[:, 2:3], ALU.mult, ALU.mult, r=[kmv, krs], w=[knm])
        self.act(src, src, AF.Identity, bias=mv[:, 3:4], scale=mv[:, 2:3], r=[hk, krs, knm], w=[hk])
        self.tt("dve", src, src, self.PB[:, 0, :], ALU.mult, r=[hk, "PBg"], w=[hk])
        self.tt("dve", src, src, self.PB[:, 1, :], ALU.add, r=[hk, "PBb"], w=[hk])
        if need_xt:
            self.cp("act", hb[:, :], src, r=[hk], w=[khb])
        if final:
            self.dma(self.y[t * 128:(t + 1) * 128, :], src, r=[hk], w=[("y", t)])
        else:
            self.act(src, src, AF.Copy, scale=ALPHA, r=[hk], w=[hk])
        if need_xt:
            if defer:
                return lambda: self.transpose_to_XT(hb, khb, t, range(8))
            self.transpose_to_XT(hb, khb, t, range(8))
        return None

    def transpose_to_XT(self, src_bf, src_key, t, kcs, src_off=0):
        kcs = list(kcs)
        bank = 3 + (self.tcount % 2)
        self.tcount += 1
        pk = ("ps", bank)
        pv = self.psum[bank][:, 0:512].bitcast(BF16).rearrange("p (k n) -> p k n", k=8)
        for i, kc in enumerate(kcs):
            self.tr(pv[:, i, :], src_bf[:, src_off + i * 128:src_off + (i + 1) * 128], self.ident_b,
                    r=[src_key, "cstb0"], w=[pk])
        self.cp("act", self.XT[:, kcs[0]:kcs[0] + len(kcs), t * 128:(t + 1) * 128], pv[:, 0:len(kcs), :],
                r=[pk], w=[("XT", t, kc) for kc in kcs])

    def gelu_to(self, dst, ps_ap, pk, n):
        sq, w1 = self.g_sq[:, 0:n], self.g_w[:, 0:n]
        self.act(sq, ps_ap, AF.Square, r=[pk], w=["g_sq"])
        self.ts("dve", w1, sq, 0.044715, 1.0, ALU.mult, ALU.add, r=["g_sq"], w=["g_w"])
        self.tt("dve", w1, w1, ps_ap, ALU.mult, r=["g_w", pk], w=["g_w"])
        self.act(sq, w1, AF.Sigmoid, scale=1.5957691216, r=["g_w"], w=["g_sq"])
        return sq

    def mixer(self, l):
        S = self.S
        self.arena_reset()
        A = self.aalloc
        NB = 1 if getattr(self, 'no_overlap', True) else 2
        kT = A([128, 3, 2048], BF16)
        Vaug = A([128, 16, 390], BF16)
        ksum_f = A([128, 3, 8], F32)
        ksum_b = A([128, 3, 8], BF16)
        kn2max = A([128, 6], F32)
        qn2max2 = [A([128, 6], F32) for _ in range(NB)]
        halo = A([128, 7, 3], F32)
        H = A([128, 384], F32)
        Hb = A([128, 384], BF16)
        WsT = A([128, 4, 128], BF16)
        normg = A([128, 1024], F32)
        gmln = A([128, 2, 256], F32)
        cw = A([128, 7, 4], F32)
        cb = A([128, 7], F32)
        bsT = A([128, 4], F32)
        sp6 = A([128, 4, 6], F32)
        NSTG = getattr(self, 'nstg', 2)
        stage = [A([128, 8, 384], BF16) for _ in range(NSTG)]
        qT2 = [A([128, 3, 256], BF16) for _ in range(NB)]
        sz2 = [A([128, 2, 384], F32) for _ in range(NB)]
        xs_tok2 = [A([128, 2, 384], F32) for _ in range(NB)]
        BT2 = [A([128, 2, 256], BF16) for _ in range(NB)]
        CT2 = [A([128, 2, 256], BF16) for _ in range(NB)]
        B_tok2 = [A([128, 2, 2, 128], BF16) for _ in range(NB)]
        att_pre = A([128, 2, 384], F32)
        dtc2 = [A([128, 2, 6], F32) for _ in range(NB)]
        ggu = A([128, 2, 256], F32)
        mixas = A([128, 2, 768], BF16)
        mixgm2 = [A([128, 2, 256], BF16) for _ in range(NB)]
        qf = A([128, 384], F32)
        rt = A([128, 4, 6, 8], F32)
        qbs = {(g_, i_): A([128, 384], BF16) for g_ in ('q', 'k') for i_ in range(2)}
        self.g_sq = A([128, 256], F32)
        sqt = A([128, 384], F32, at=self.last_off)
        self.g_w = A([128, 256], F32)
        ggv2 = A([128, 2, 256], F32)
        gmp2 = A([128, 2, 256], F32)
        vln2 = A([128, 2, 256], BF16)
        raw2 = [A([128, 259], F32) for _ in range(2)]
        Wsb = A([128, 4, 128], BF16, at=self.last_off - 1036)
        cacc2 = [A([128, 256], F32) for _ in range(2)]
        xsT3 = A([128, 3, 256], F32)
        sm = A([128, 128], F32)
        a_bc2 = A([128, 2, 3, 128], F32)
        seg2 = A([128, 2, 3, 128], F32)
        MT2 = A([128, 2, 3, 128], BF16)
        xdt2 = A([128, 2, 384], BF16)
        xdtd2 = A([128, 2, 384], BF16)
        ytmp2 = A([128, 2, 384], F32)
        yo = A([128, 384], F32)
        yy = A([128, 384], F32)
        ET = [A([128, 256], BF16) for _ in range(3)]
        gsel = A([128, 2, 6, 8], F32)
        acc = A([128, 2, 65], F32)
        atmp = A([128, 2, 65], F32)
        gtmp = A([128, 6, 8], F32)
        gtop = A([128, 6, 8], F32)
        st12 = A([128, 16], F32)
        st12b = A([128, 16], F32)
        self.g_lnst = A([128, 2, 6], F32)
        self.g_lnmv = A([128, 4], F32)
        dgt = A([128, 16], F32)
        negM = A([128, 8], F32)
        rden = A([128, 4], F32)
        ssq = A([128, 16], F32)
        PS = self.psum
        CK = "cstf"

        self.dma(normg[:, :], self.normg[l, :].partition_broadcast(128), w=["normg"])
        self.dma(gmln[:, 0, :], self.gm_ln_g[l, :].partition_broadcast(128), w=["gmln0"])
        self.dma(gmln[:, 1, :], self.gm_ln_b[l, :].partition_broadcast(128), w=["gmln1"])
        self.dma(cw[:, :, :], self.conv_wT[l, :, :, :], w=["cw"])
        self.dma(cb[:, :], self.conv_bT[l, :, :], w=["cb"])
        self.dma(bsT[:, :], self.gm_b_sT[l, :, :], w=["bsT"])
        self.dma(sp6[:, 0, :], self.dt_bias[l, :].partition_broadcast(128), w=["dtb"])
        self.dma(sp6[:, 1, :], self.a_log[l, :].partition_broadcast(128), w=["alog"])
        self.dma(sp6[:, 2, :], self.d_skip[l, :].partition_broadcast(128), w=["dsk"])
        self.act(sp6[:, 3, :], sp6[:, 1, :], AF.Exp, r=["alog"], w=["Aneg"])
        self.ts("dve", sp6[:, 3, :], sp6[:, 3, :], -1.0, None, ALU.mult, r=["Aneg"], w=["Aneg"])
        self.dma(Wsb[:, :, :], self.gm_w_s[l, :, :, :].rearrange("g t s -> t g s"), w=[("raw", 0), ("rawh", 0)], eng="pool", lane="pw2")
        self.tt("dve", Wsb[:, :, :], Wsb[:, :, :], self.tril_f.unsqueeze(1).to_broadcast([128, 4, 128]), ALU.mult,
                r=[("raw", 0), ("rawh", 0), CK], w=[("raw", 0), ("rawh", 0)])
        pv = PS[3][:, 0:256].bitcast(BF16).rearrange("p (g n) -> p g n", g=4)
        for g in range(4):
            self.tr(pv[:, g, :], Wsb[:, g, :], self.ident_b, r=[("raw", 0), ("rawh", 0), "cstb0"], w=[("ps", 3)])
        self.cp("dve", WsT[:, :, :], pv, r=[("ps", 3)], w=["WsT"])
        self.memset("pool", Vaug[:, :, :], 1.0, w=[("V", t) for t in range(NT)])
        self.memset("pool", H[:, :], 0.0, w=["H"])
        self.memset("pool", Hb[:, :], 0.0, w=["Hb"])
        self.memset("pool", halo[:, :, :], 0.0, w=[("halo", j) for j in range(7)])
        self.memset("pool", kn2max[:, :], 0.0, w=["kn2max"])
        self.memset("pool", ksum_f[:, :, :], 0.0, w=["ksum_f"])

        groups = [("q", 0, 384), ("k", 384, 384), ("v", 768, 384), ("z", 1152, 384), ("xa", 1536, 384),
                  ("xb", 1920, 384), ("xc", 2304, 134), ("gu", 2438, 256), ("gv", 2694, 256)]
        nchunks = getattr(self, "mixer_chunks", 8)
        stage_n = [0]
        bank_n = [0]

        deferred = []

        def next_bank():
            bank_n[0] += 1
            return bank_n[0] % 2

        cos = self.cs[:, 0, :, :]
        sin = self.cs[:, 1, :, :]

        def rope(buf, key, t):
            v = buf[:, :].rearrange("p (h d) -> p h d", h=6)
            x1, x2 = v[:, :, 0:8], v[:, :, 8:16]
            c = cos[:, t:t + 1, :].to_broadcast([128, 6, 8])
            s_ = sin[:, t:t + 1, :].to_broadcast([128, 6, 8])
            self.tt("dve", rt[:, 0, :, :], x1, c, ALU.mult, r=[key, ("cs", 0)], w=["rt0"])
            self.tt("dve", rt[:, 1, :, :], x2, s_, ALU.mult, r=[key, ("cs", 1)], w=["rt1"])
            self.tt("dve", rt[:, 2, :, :], x2, c, ALU.mult, r=[key, ("cs", 0)], w=["rt2"])
            self.tt("dve", rt[:, 3, :, :], x1, s_, ALU.mult, r=[key, ("cs", 1)], w=["rt3"])
            self.tt("dve", x1, rt[:, 0, :, :], rt[:, 1, :, :], ALU.subtract, r=["rt0", "rt1"], w=[key])
            self.tt("dve", x2, rt[:, 2, :, :], rt[:, 3, :, :], ALU.add, r=["rt2", "rt3"], w=[key])

        def proj_gen(c):
            p = c % NB
            qT, sz, xs_tok, BT, CT, B_tok, dtc, mixp, qn2max = (qT2[p], sz2[p], xs_tok2[p], BT2[p], CT2[p], B_tok2[p],
                                                                dtc2[p], None, qn2max2[p])
            tiles = [2 * c, 2 * c + 1]
            ccols = slice(c * 256, (c + 1) * 256)
            for (gname, c0, ncol) in groups:
                yield
                prev_def = deferred[:]
                del deferred[:]
                self.S.tag = "m%d.c%d.proj_%s" % (l, c, gname)
                sidx = stage_n[0] % NSTG
                stage_n[0] += 1
                stg = stage[sidx]
                sk = ("stage", sidx)
                self.dma(stg[:, :, 0:ncol], self.w_in_bf[l, :, c0:c0 + ncol].rearrange("(kc p) n -> p kc n", p=128),
                         r=[("w_in_bf", l)], w=[sk], eng="sp", lane="st%d" % sidx)
                if gname in ("q", "k", "v", "z", "gu", "gv"):
                    for i, t in enumerate(tiles):
                        if i == 1:
                            yield
                        pb = next_bank()
                        pk = ("ps", pb)
                        ps = PS[pb][:, 0:ncol]
                        for kc in range(8):
                            self.mm(ps, self.XT[:, kc, t * 128:(t + 1) * 128], stg[:, kc, 0:ncol], start=(kc == 0),
                                    stop=(kc == 7), r=[("XT", t, kc), sk], w=[pk])
                        if i == 0:
                            for fn_ in prev_def:
                                fn_()
                            prev_def = []
                        if gname in ("q", "k"):
                            buf, bk = (qf, "qf")
                            self.act(buf[:, :], ps, AF.Copy, scale=(0.125 if gname == "q" else 1.0), r=[pk], w=[bk])
                            rope(buf, bk, t)
                            qb = qbs[(gname, i)]
                            qbk = ("qb", gname, i)
                            self.cp("act", qb[:, :], buf[:, :], r=[bk], w=[qbk])
                            self.tt("dve", sqt[:, :], buf[:, :], buf[:, :], ALU.mult, r=[bk], w=["g_sq", "g_w"])
                            if gname == "q":
                                dstn, dk = st12[:, 0:6], "qn2"
                            else:
                                dstn, dk = st12[:, 8:14], "kn2"
                            S.add("dve", (lambda dstn: lambda e: e.tensor_reduce(
                                out=dstn, in_=sqt[:, :].rearrange("p (h d) -> p h d", h=6), axis=AX.X, op=ALU.add))(dstn),
                                r=["g_sq", "g_w"], w=[dk])
                            if gname == "q":
                                if i == 0:
                                    self.cp("dve", qn2max[:, :], dstn, r=[dk], w=[("qn2max", p)])
                                else:
                                    self.tt("dve", qn2max[:, :], qn2max[:, :], dstn, ALU.max, r=[dk, ("qn2max", p)], w=[("qn2max", p)])
                            else:
                                self.tt("dve", kn2max[:, :], kn2max[:, :], dstn, ALU.max, r=[dk, "kn2max"], w=["kn2max"])

                            def _tr(gname=gname, i=i, t=t, qb=qb, qbk=qbk, c=c, tiles=tiles):
                                tb = 3
                                self.tcount += 1
                                tk = ("ps", tb)
                                tv = PS[tb][:, 0:192].bitcast(BF16).rearrange("p (k n) -> p k n", k=3)
                                for pr in range(3):
                                    self.tr(tv[:, pr, :], qb[:, pr * 128:(pr + 1) * 128], self.ident_b, r=[qbk, "cstb0"], w=[tk])
                                if gname == "q":
                                    self.cp("dve", qT[:, :, i * 128:(i + 1) * 128], tv, r=[tk], w=[("qT", p, i)])
                                else:
                                    self.cp("dve", kT[:, :, t * 128:(t + 1) * 128], tv, r=[tk], w=[("kT", t)])
                                    if i == 1:
                                        S.add("dve", (lambda c: lambda e: e.tensor_reduce(
                                            out=ksum_f[:, :, c], in_=kT[:, :, c * 256:(c + 1) * 256], axis=AX.X, op=ALU.add))(c),
                                            r=[("kT", tiles[0]), ("kT", tiles[1])], w=["ksum_f"])
                                        self.cp("dve", ksum_b[:, :, :], ksum_f[:, :, :], r=["ksum_f"], w=["ksum_b"])
                            deferred.append(_tr)
                        elif gname == "v":
                            vv = Vaug[:, t, :].rearrange("p (h e) -> p h e", e=65)[:, :, 0:64]
                            self.act(vv, ps.rearrange("p (h d) -> p h d", h=6), AF.Copy, r=[pk], w=[("V", t)])
                        elif gname == "z":
                            self.act(sz[:, i, :], ps, AF.Silu, r=[pk], w=[("sz", p, i)])
                        elif gname == "gu":
                            sg = self.gelu_to(None, ps, pk, 256)
                            self.tt("dve", ggu[:, i, :], sg, ps, ALU.mult, r=["g_sq", pk], w=[("ggu", i)])
                        elif gname == "gv":
                            sg = self.gelu_to(None, ps, pk, 256)
                            self.tt("dve", ggv2[:, i, :], sg, ps, ALU.mult, r=["g_sq", pk], w=[("ggv", i)])
                            if i == 1:
                                self.gmlp_chunk(l, ggv2, vln2, gmp2, ggu, WsT, bsT, gmln, normg, mixgm2[p], ssq, p)
                else:
                    njl = {"xa": 3, "xb": 3, "xc": 1}[gname]
                    j0 = {"xa": 0, "xb": 3, "xc": 6}[gname]
                    for jl in range(njl):
                        j = j0 + jl
                        xb_ = 2 if j % 2 == 0 else 4
                        pk = ("ps", xb_)
                        ps = PS[xb_][:, 0:256]
                        for kc in range(8):
                            self.mm(ps, stg[:, kc, jl * 128:(jl + 1) * 128], self.XT[:, kc, ccols], start=(kc == 0),
                                    stop=(kc == 7), r=[("XT", tiles[0], kc), ("XT", tiles[1], kc), sk], w=[pk])
                        if jl == 0:
                            for fn_ in prev_def:
                                fn_()
                            prev_def = []
                        self.conv_chunk(j, ps, pk, raw2[j % 2], halo, cw, cb, cacc2[j % 2], xsT3, xs_tok, BT, CT, B_tok, deferred, p)
                    if gname == "xc":
                        pk = ("ps", 3)
                        for i, t in enumerate(tiles):
                            for kc in range(8):
                                self.mm(PS[3][:, i * 8:i * 8 + 6], self.XT[:, kc, t * 128:(t + 1) * 128], stg[:, kc, 128:134],
                                        start=(kc == 0), stop=(kc == 7), r=[("XT", t, kc), sk], w=[pk])
                        xv = sm[:, 0:12].rearrange("p (i h) -> p i h", i=2)
                        ax = sm[:, 12:24].rearrange("p (i h) -> p i h", i=2)
                        self.tt("dve", xv, PS[3][:, 0:16].rearrange("p (i h) -> p i h", i=2)[:, :, 0:6],
                                sp6[:, 0:1, :].to_broadcast([128, 2, 6]), ALU.add, r=[pk, "dtb"], w=["dtx"])
                        self.stt("dve", ax, xv, -1.0, xv, ALU.mult, ALU.max, r=["dtx"], w=["dtax"])
                        self.act(ax, ax, AF.Exp, scale=-1.0, r=["dtax"], w=["dtax"])
                        self.act(ax, ax, AF.Ln, bias=1.0, r=["dtax"], w=["dtax"])
                        self.stt("dve", dtc[:, :, :], xv, 0.0, ax, ALU.max, ALU.add, r=["dtx", "dtax"], w=[("dtc", p)])
            for fn_ in deferred:
                fn_()
            del deferred[:]

        def back_gen(c):
            p = c % NB
            qT, sz, xs_tok, BT, CT, B_tok, dtc, mixp, qn2max = (qT2[p], sz2[p], xs_tok2[p], BT2[p], CT2[p], B_tok2[p],
                                                                dtc2[p], None, qn2max2[p])
            tiles = [2 * c, 2 * c + 1]
            self.S.tag = "m%d.c%d.ssd" % (l, c)
            yield
            self.ssd_chunk(l, c, p, dict(sm=sm, sp6=sp6, dtc=dtc, xs_tok=xs_tok, BT=BT, CT=CT, B_tok=B_tok, a_bc2=a_bc2,
                                         seg2=seg2, MT2=MT2, xdt2=xdt2, xdtd2=xdtd2, ytmp2=ytmp2, yo=yo, yy=yy, H=H, Hb=Hb, sz=sz,
                                         normg=normg, mixp=mixas, ssq=ssq))
            self.S.tag = "m%d.c%d.att" % (l, c)
            yield from self.attention_chunk(l, c, p, dict(kT=kT, qT=qT, Vaug=Vaug, ksum_b=ksum_b, kn2max=kn2max, qn2max=qn2max, st12=st12,
                                            st12b=st12b, dgt=dgt, negM=negM, ET=ET, gsel=gsel, acc=acc, atmp=atmp, gtmp=gtmp, gtop=gtop,
                                            att_pre=att_pre, rden=rden, ssq=ssq, normg=normg, mixp=mixas, sqt=yo))
            self.S.tag = "m%d.c%d.mixT" % (l, c)
            for i, t in enumerate(tiles):
                yield
                keys = [("mixp", "att", i), ("mixp", "ssd", i), ("mixp", p, "gm", i)]
                bank = 4 + (self.tcount % 2)
                self.tcount += 1
                pk = ("ps", bank)
                pvv = PS[bank][:, 0:512].bitcast(BF16).rearrange("p (k n) -> p k n", k=8)
                for kc in range(8):
                    srcm = mixas[:, i, kc * 128:(kc + 1) * 128] if kc < 6 else mixgm2[p][:, i, (kc - 6) * 128:(kc - 5) * 128]
                    self.tr(pvv[:, kc, :], srcm, self.ident_b, r=keys + ["cstb0"], w=[pk])
                self.cp("dve", self.XT[:, :, t * 128:(t + 1) * 128], pvv, r=[pk], w=self.xtk(t))


        def drain(g):
            for _ in g:
                pass

        def merge(g1, n1, g2, n2):
            a1 = a2 = True
            e = 0.0
            while a1 or a2:
                pick1 = a1 and (not a2 or e <= 0)
                if pick1:
                    try:
                        next(g1)
                        e += float(n2) / max(n1, 1)
                    except StopIteration:
                        a1 = False
                else:
                    try:
                        next(g2)
                        e -= 1.0
                    except StopIteration:
                        a2 = False

        drain(proj_gen(0))
        for c in range(nchunks):
            nb_ = 8 + 6 * (2 * c + 2) + (6 * c if c >= 4 else 0)
            if c + 1 < nchunks and not getattr(self, "no_overlap", True):
                merge(back_gen(c), nb_, proj_gen(c + 1), 19)
            else:
                drain(back_gen(c))
                if c + 1 < nchunks:
                    drain(proj_gen(c + 1))
        if self.stop in ("proj", "ssd", "att"):
            self.dump("mixp", mixas[:, :, :], r=[("mixp", pc, i) for pc in ("att", "ssd") for i in range(2)])
            pl = (nchunks - 1) % NB
            self.dump("mixgm", mixgm2[pl][:, :, :], r=[("mixp", pl, "gm", i) for i in range(2)])
            self.dump("dtc", dtc2[pl][:, :, :], r=[("dtc", pl)])
            return

    def rstd_from(self, dst, src, inv_n, rk, wk):
        self.ts("dve", dst, src, float(inv_n), EPS, ALU.mult, ALU.add, r=rk, w=[wk])
        n = dst.shape[1]
        self.tt("pool", dst, dst, self.cstf[:, 712:713].to_broadcast([128, n]), ALU.pow, r=[wk, "cstf"], w=[wk])

    def gmlp_tile(self, l, i, t, ggv, vln, gmp, ggu, WsT, bsT, gmln, normg, mixp, ssq, p):
        S = self.S
        st, mv = self.g_lnst, self.g_lnmv
        S.add("dve", lambda e: e.bn_stats(out=st[:, 0, :], in_=ggv[:, :]), r=["ggv"], w=["g_lnst"])
        S.add("dve", lambda e: e.bn_aggr(out=mv[:, 0:2], in_=st[:, 0:1, :]), r=["g_lnst"], w=["g_lnmv"])
        self.ts("dve", mv[:, 2:3], mv[:, 1:2], EPS, None, ALU.add, r=["g_lnmv"], w=["g_lnrs"])
        self.tt("pool", mv[:, 2:3], mv[:, 2:3], self.cstf[:, 712:713], ALU.pow, r=["g_lnrs", "cstf"], w=["g_lnrs"])
        self.stt("dve", mv[:, 3:4], mv[:, 0:1], -1.0, mv[:, 2:3], ALU.mult, ALU.mult, r=["g_lnmv", "g_lnrs"], w=["g_lnnm"])
        self.act(ggv[:, :], ggv[:, :], AF.Identity, bias=mv[:, 3:4], scale=mv[:, 2:3], r=["ggv", "g_lnrs", "g_lnnm"], w=["ggv"])
        self.tt("dve", ggv[:, :], ggv[:, :], gmln[:, 0, :], ALU.mult, r=["ggv", "gmln0"], w=["ggv"])
        self.tt("dve", vln[:, :], ggv[:, :], gmln[:, 1, :], ALU.add, r=["ggv", "gmln1"], w=["vln"])
        pk = ("ps", 2)
        ps = self.psum[2][:, 0:256]
        for g in range(4):
            self.mm(ps[:, g * 64:(g + 1) * 64], WsT[:, g, :], vln[:, g * 64:(g + 1) * 64], r=["WsT", "vln"], w=[pk])
        for g in range(4):
            self.stt("dve", gmp[:, g * 64:(g + 1) * 64], ps[:, g * 64:(g + 1) * 64], bsT[:, g:g + 1], ggu[:, i, g * 64:(g + 1) * 64],
                     ALU.add, ALU.mult, r=[pk, "bsT", ("ggu", i)], w=["g_w"])
        self.memset("dve", ssq[:, 0:1], 0.0, w=["ssq0"])
        self.act(ggv[:, :], gmp[:, :], AF.Square, accum=ssq[:, 0:1], r=["g_w", "ssq0"], w=["ggv", "ssq0"])
        self.rstd_from(ssq[:, 1:2], ssq[:, 0:1], 1.0 / 256.0, ["ssq0"], "ssq1")
        self.stt("dve", mixp[:, i, :], gmp[:, :], ssq[:, 1:2], normg[:, 768:1024], ALU.mult, ALU.mult,
                 r=["g_w", "ssq1", "normg"], w=[("mixp", p, "gm", i)])

    def gmlp_chunk(self, l, ggv, vln, gmp, ggu, WsT, bsT, gmln, normg, mixp, ssq, p):
        S = self.S
        st, mv = self.g_lnst, self.g_lnmv
        rs = ssq[:, 8:12]
        for i in range(2):
            S.add("dve", (lambda i: lambda e: e.bn_stats(out=st[:, i, :], in_=ggv[:, i, :]))(i), r=[("ggv", i)], w=[("g_lnst", i)])
            S.add("dve", (lambda i: lambda e: e.bn_aggr(out=mv[:, 2 * i:2 * i + 2], in_=st[:, i:i + 1, :]))(i),
                  r=[("g_lnst", i)], w=["g_lnmv"])
        mvv = mv[:, 0:4].rearrange("p (i s) -> p i s", i=2)
        self.ts("dve", rs[:, 0:2], mvv[:, :, 1], EPS, None, ALU.add, r=["g_lnmv"], w=["g_lnrs"])
        self.tt("pool", rs[:, 0:2], rs[:, 0:2], self.cstf[:, 712:713].to_broadcast([128, 2]), ALU.pow, r=["g_lnrs", "cstf"], w=["g_lnrs"])
        self.stt("dve", rs[:, 2:4], mvv[:, :, 0], -1.0, rs[:, 0:2], ALU.mult, ALU.mult, r=["g_lnmv", "g_lnrs"], w=["g_lnnm"])
        for i in range(2):
            self.act(ggv[:, i, :], ggv[:, i, :], AF.Identity, bias=rs[:, 2 + i:3 + i], scale=rs[:, i:i + 1],
                     r=[("ggv", i), "g_lnrs", "g_lnnm"], w=[("ggv", i)])
        gk = [("ggv", 0), ("ggv", 1)]
        self.tt("dve", ggv[:, :, :], ggv[:, :, :], gmln[:, 0:1, :].to_broadcast([128, 2, 256]), ALU.mult, r=gk + ["gmln0"], w=gk)
        self.tt("dve", vln[:, :, :], ggv[:, :, :], gmln[:, 1:2, :].to_broadcast([128, 2, 256]), ALU.add, r=gk + ["gmln1"], w=["vln"])
        pk = ("ps", 2)
        ps = self.psum[2][:, 0:512].rearrange("p (i n) -> p i n", i=2)
        for i in range(2):
            for g in range(4):
                self.mm(ps[:, i, g * 64:(g + 1) * 64], WsT[:, g, :], vln[:, i, g * 64:(g + 1) * 64], r=["WsT", "vln"], w=[pk])
        for i in range(2):
            self.tt("dve", gmp[:, i, :].rearrange("p (g d) -> p g d", g=4), ps[:, i, :].rearrange("p (g d) -> p g d", g=4),
                    bsT[:, 0:4].unsqueeze(2).to_broadcast([128, 4, 64]), ALU.add, r=[pk, "bsT"], w=["gmp"])
        self.tt("dve", gmp[:, :, :], gmp[:, :, :], ggu[:, :, :], ALU.mult, r=["gmp", ("ggu", 0), ("ggu", 1)], w=["gmp"])
        self.memset("dve", ssq[:, 0:2], 0.0, w=["ssq0"])
        for i in range(2):
            self.act(ggv[:, i, :], gmp[:, i, :], AF.Square, accum=ssq[:, i:i + 1], r=["gmp", "ssq0"], w=[("ggv", i), "ssq0"])
        self.rstd_from(ssq[:, 12:14], ssq[:, 0:2], 1.0 / 256.0, ["ssq0"], "ssq1")
        for i in range(2):
            self.stt("dve", mixp[:, i, :], gmp[:, i, :], ssq[:, 12 + i:13 + i], normg[:, 768:1024], ALU.mult, ALU.mult,
                     r=["gmp", "ssq1", "normg"], w=[("mixp", p, "gm", i)])

    def conv_chunk(self, j, ps, pk, raw, halo, cw, cb, cacc, xsT3, xs_tok, BT, CT, B_tok, deferred, p):
        self.act(raw[:, 3:259], ps, AF.Copy, r=[pk], w=[("raw", j % 2)])
        self.cp("pool", raw[:, 0:3], halo[:, j, :], r=[("halo", j)], w=[("rawh", j % 2)])
        self.ts("dve", cacc[:, :], raw[:, 3:259], cw[:, j, 3:4], cb[:, j:j + 1], ALU.mult, ALU.add, r=[("raw", j % 2), "cw", "cb"], w=[("cacc", j % 2)])
        for i in range(3):
            self.stt("dve", cacc[:, :], raw[:, i:i + 256], cw[:, j, i:i + 1], cacc[:, :], ALU.mult, ALU.add,
                     r=[("raw", j % 2), ("rawh", j % 2), "cw", ("cacc", j % 2)], w=[("cacc", j % 2)])
        self.cp("pool", halo[:, j, :], raw[:, 256:259], r=[("raw", j % 2)], w=[("halo", j)])
        if j < 3:
            xk = "xsT%d" % j
            self.act(xsT3[:, j, :], cacc[:, :], AF.Silu, r=[("cacc", j % 2)], w=[xk])

            def _tr(j=j, xk=xk):
                bank = 3
                tk = ("ps", bank)
                tv = self.psum[bank][:, 0:256].rearrange("p (i n) -> p i n", i=2)
                for i in range(2):
                    self.tr(tv[:, i, :], xsT3[:, j, i * 128:(i + 1) * 128], self.ident_f, r=[xk, "cstf"], w=[tk])
                self.cp("dve", xs_tok[:, :, j * 128:(j + 1) * 128], tv, r=[tk], w=[("xs", p, 0), ("xs", p, 1)])
            deferred.append(_tr)
        elif j < 5:
            g = j - 3
            self.act(BT[:, g, :], cacc[:, :], AF.Silu, r=[("cacc", j % 2)], w=[("BT", p, g)])

            def _tr(g=g):
                bank = 3
                tk = ("ps", bank)
                tv = self.psum[bank][:, 0:128].bitcast(BF16).rearrange("p (i n) -> p i n", i=2)
                for i in range(2):
                    self.tr(tv[:, i, :], BT[:, g, i * 128:(i + 1) * 128], self.ident_b, r=[("BT", p, g), "cstb0"], w=[tk])
                self.cp("dve", B_tok[:, :, g, :], tv, r=[tk], w=[("Btok", p)])
            deferred.append(_tr)
        else:
            g = j - 5
            self.act(CT[:, g, :], cacc[:, :], AF.Silu, r=[("cacc", j % 2)], w=[("CT", p, g)])

    def ssd_tile(self, l, i, t, c, p, B):
        S = self.S
        PS = self.psum
        sm, sp6, dtc, xs_tok = B["sm"], B["sp6"], B["dtc"], B["xs_tok"]
        BT, CT, B_tok, a_bc, seg, MT = B["BT"], B["CT"], B["B_tok"], B["a_bc"], B["seg"], B["MT"]
        xdt, xdtd, yo, yy, ytmp, H, Hb, sz = B["xdt"], B["xdtd"], B["yo"], B["yy"], B["ytmp"], B["H"], B["Hb"], B["sz"]
        normg, mixp, ssq = B["normg"], B["mixp"], B["ssq"]
        tc = slice(i * 128, (i + 1) * 128)
        dt = dtc[:, i, :]
        a = sm[:, 24:30]
        acol = sm[:, 30:36]
        ea = sm[:, 36:42]
        dec = sm[:, 42:48]
        cdec = sm[:, 48:54]
        xs = xs_tok[:, i, :]
        xs3 = xs.rearrange("p (h d) -> p h d", h=6)

        def b3(v):
            return v.unsqueeze(2).to_broadcast([128, 6, 64])

        def v3(ap):
            return ap.rearrange("p (h d) -> p h d", h=6)

        self.tt("dve", a, dt, sp6[:, 3, :], ALU.mult, r=[("dtc", p), "Aneg"], w=["ssd_a"])
        k5 = ("ps", 4)
        self.mm(PS[4][:, 32:38], self.U_f, a, r=["cstf", "ssd_a"], w=[k5])
        self.mm(PS[4][:, 40:46], self.ones_f, a, r=["cstf", "ssd_a"], w=[k5])
        self.cp("dve", acol, PS[4][:, 32:38], r=[k5], w=["acol"])
        self.act(ea, acol, AF.Exp, r=["acol"], w=["ea"])
        self.tt("dve", dec, PS[4][:, 40:46], acol, ALU.subtract, r=[k5, "acol"], w=["dec"])
        self.act(dec, dec, AF.Exp, r=["dec"], w=["dec"])
        self.act(cdec, PS[4][:, 40:46], AF.Exp, r=[k5], w=["cdec"])
        self.tt("dve", v3(ytmp[:, :]), xs3, b3(dt), ALU.mult, r=[("xs", p, i), ("dtc", p)], w=["ytmp"])
        self.cp("act", xdt[:, :], ytmp[:, :], r=["ytmp"], w=["xdt"])
        self.tt("dve", v3(xdtd[:, :]), v3(ytmp[:, :]), b3(dec), ALU.mult, r=["ytmp", "dec"], w=["xdtd"])
        k6, k7, k2 = ("ps", 4), ("ps", 5), ("ps", 5)
        k0l, k1l = self.pkeys(6, 0, 384), self.pkeys(7, 0, 384)
        for gi in range(2):
            self.mm(PS[4][:, 256 + gi * 128:256 + (gi + 1) * 128], BT[:, gi, tc], CT[:, gi, tc], r=[("BT", p, gi), ("CT", p, gi)], w=[k6])
        for gi in range(2):
            hs = range(3 * gi, 3 * gi + 3)
            self.cp("dve", a_bc[:, :, :], a[:, 3 * gi:3 * gi + 3].unsqueeze(2).to_broadcast([128, 3, 128]), r=["ssd_a"], w=["a_bc"])
            p7 = PS[5][:, 0:384].rearrange("p (h n) -> p h n", h=3)
            for hl in range(3):
                self.mm(p7[:, hl, :], a_bc[:, hl, :], self.U_f, r=["a_bc", "cstf"], w=[k7])
            for hl, h in enumerate(hs):
                self.stt("dve", seg[:, hl, :], p7[:, hl, :], acol[:, h:h + 1], self.negm_f, ALU.subtract, ALU.add,
                         r=[k7, "acol", "cstf"], w=["seg"])
            self.act(seg[:, :, :], seg[:, :, :], AF.Exp, r=["seg"], w=["seg"])
            self.tt("dve", MT[:, :, :], seg[:, :, :], PS[4][:, 256 + gi * 128:256 + (gi + 1) * 128].unsqueeze(1).to_broadcast([128, 3, 128]),
                    ALU.mult, r=["seg", k6], w=["MT"])
            for hl, h in enumerate(hs):
                self.mm(PS[6][:, h * 64:(h + 1) * 64], MT[:, hl, :], xdt[:, h * 64:(h + 1) * 64], r=["MT", "xdt"], w=k0l)
        for gi in range(2):
            gc = slice(gi * 192, (gi + 1) * 192)
            self.mm(PS[7][:, gc], CT[:, gi, tc], Hb[:, gc], r=[("CT", p, gi), "Hb"], w=k1l)
            self.mm(PS[5][:, gc], B_tok[:, i, gi, :], xdtd[:, gc], r=[("Btok", p), "xdtd"], w=[k2])
        self.tt("dve", v3(yo[:, :]), v3(PS[7][:, 0:384]), b3(ea), ALU.mult, r=k1l + ["ea"], w=["yo"])
        self.tt("dve", yy[:, :], PS[6][:, 0:384], yo[:, :], ALU.add, r=k0l + ["yo"], w=["yy"])
        self.tt("dve", v3(ytmp[:, :]), xs3, b3(sp6[:, 2, :]), ALU.mult, r=[("xs", p, i), "dsk"], w=["ytmp"])
        self.tt("dve", yy[:, :], yy[:, :], ytmp[:, :], ALU.add, r=["yy", "ytmp"], w=["yy"])
        self.tt("dve", yy[:, :], yy[:, :], sz[:, i, :], ALU.mult, r=["yy", ("sz", p, i)], w=["yy"])
        self.memset("dve", ssq[:, 2:4], 0.0, w=["ssq2"])
        for gi in range(2):
            gc = slice(gi * 192, (gi + 1) * 192)
            self.act(yo[:, gc], yy[:, gc], AF.Square, accum=ssq[:, 2 + gi:3 + gi], r=["yy", "ssq2"], w=["yo", "ssq2"])
        self.rstd_from(ssq[:, 4:6], ssq[:, 2:4], 1.0 / 192.0, ["ssq2"], "ssq4")
        for gi in range(2):
            gc = slice(gi * 192, (gi + 1) * 192)
            self.stt("dve", mixp[:, i, 384 + gi * 192:384 + (gi + 1) * 192], yy[:, gc], ssq[:, 4 + gi:5 + gi],
                     normg[:, 384 + gi * 192:384 + (gi + 1) * 192], ALU.mult, ALU.mult,
                     r=["yy", "ssq4", "normg"], w=[("mixp", "ssd", i)])
        self.tt("dve", v3(ytmp[:, :]), v3(H[:, :]), b3(cdec), ALU.mult, r=["H", "cdec"], w=["ytmp"])
        self.tt("dve", H[:, :], ytmp[:, :], PS[5][:, 0:384], ALU.add, r=["ytmp", k2], w=["H"])
        self.cp("act", Hb[:, :], H[:, :], r=["H"], w=["Hb"])

    def ssd_chunk(self, l, c, p, B):
        S = self.S
        PS = self.psum
        sm, sp6, dtc, xs_tok = B["sm"], B["sp6"], B["dtc"], B["xs_tok"]
        BT, CT, B_tok = B["BT"], B["CT"], B["B_tok"]
        a_bc, seg, MT, xdt, xdtd, ytmp2 = B["a_bc2"], B["seg2"], B["MT2"], B["xdt2"], B["xdtd2"], B["ytmp2"]
        yo, yy, H, Hb, sz = B["yo"], B["yy"], B["H"], B["Hb"], B["sz"]
        normg, mixp, ssq = B["normg"], B["mixp"], B["ssq"]
        a2 = sm[:, 64:76]
        acol = sm[:, 76:88]
        ea = sm[:, 88:100]
        dec = sm[:, 100:112]
        cdec = sm[:, 112:124]
        t2 = lambda v: v.rearrange("p (i h) -> p i h", i=2)
        k4, k0 = ("ps", 4), ("ps", 0)

        def v4(ap):
            return ap.rearrange("p i (h d) -> p i h d", h=6)

        def b4(v):
            return v.unsqueeze(3).to_broadcast([128, 2, 6, 64])

        def v3(ap):
            return ap.rearrange("p (h d) -> p h d", h=6)

        def b3(v):
            return v.unsqueeze(2).to_broadcast([128, 6, 64])

        self.tt("dve", t2(a2), dtc[:, :, :], sp6[:, 3:4, :].to_broadcast([128, 2, 6]), ALU.mult, r=[("dtc", p), "Aneg"], w=["ssd_a"])
        self.mm(PS[4][:, 32:44], self.U_f, a2, r=["cstf", "ssd_a"], w=[k4])
        self.mm(PS[4][:, 48:60], self.ones_f, a2, r=["cstf", "ssd_a"], w=[k4])
        self.cp("dve", acol, PS[4][:, 32:44], r=[k4], w=["acol"])
        self.act(ea, acol, AF.Exp, r=["acol"], w=["ea"])
        self.tt("dve", dec, PS[4][:, 48:60], acol, ALU.subtract, r=[k4, "acol"], w=["dec"])
        self.act(dec, dec, AF.Exp, r=["dec"], w=["dec"])
        self.act(cdec, PS[4][:, 48:60], AF.Exp, r=[k4], w=["cdec"])
        self.tt("dve", v4(ytmp2[:, :, :]), v4(xs_tok[:, :, :]), b4(dtc[:, :, :]), ALU.mult,
                r=[("xs", p, 0), ("xs", p, 1), ("dtc", p)], w=["ytmp2"])
        self.cp("act", xdt[:, :, :], ytmp2[:, :, :], r=["ytmp2"], w=["xdt"])
        self.tt("dve", v4(xdtd[:, :, :]), v4(ytmp2[:, :, :]), b4(t2(dec)), ALU.mult, r=["ytmp2", "dec"], w=["xdtd"])
        cb4 = PS[0][:, 0:512].rearrange("p (i g n) -> p i g n", i=2, g=2)
        for i in range(2):
            tc = slice(i * 128, (i + 1) * 128)
            for gi in range(2):
                self.mm(cb4[:, i, gi, :], BT[:, gi, tc], CT[:, gi, tc], r=[("BT", p, gi), ("CT", p, gi)], w=[k0])
        rb = [PS[5][:, 0:384].rearrange("p (h n) -> p h n", h=3), PS[1][:, 0:384].rearrange("p (h n) -> p h n", h=3)]
        rk = [("ps", 5), ("ps", 1)]
        yk = [("ps", 6), ("ps", 7)]
        for gi in range(2):
            self.cp("dve", a_bc[:, :, :, :], t2(a2)[:, :, 3 * gi:3 * gi + 3].unsqueeze(3).to_broadcast([128, 2, 3, 128]),
                    r=["ssd_a"], w=["a_bc"])
            for i in range(2):
                for hl in range(3):
                    self.mm(rb[i][:, hl, :], a_bc[:, i, hl, :], self.U_f, r=["a_bc", "cstf"], w=[rk[i]])
            for i in range(2):
                for hl in range(3):
                    h = 3 * gi + hl
                    self.stt("dve", seg[:, i, hl, :], rb[i][:, hl, :], acol[:, i * 6 + h:i * 6 + h + 1], self.negm_f,
                             ALU.subtract, ALU.add, r=[rk[i], "acol", "cstf"], w=["seg"])
            self.act(seg[:, :, :, :], seg[:, :, :, :], AF.Exp, r=["seg"], w=["seg"])
            self.tt("dve", MT[:, :, :, :], seg[:, :, :, :], cb4[:, :, gi, :].unsqueeze(2).to_broadcast([128, 2, 3, 128]),
                    ALU.mult, r=["seg", k0], w=["MT"])
            for i in range(2):
                for hl in range(3):
                    h = 3 * gi + hl
                    self.mm(PS[6 + i][:, h * 64:(h + 1) * 64], MT[:, i, hl, :], xdt[:, i, h * 64:(h + 1) * 64],
                            r=["MT", "xdt"], w=[yk[i]])
        k2, k3 = ("ps", 2), ("ps", 3)
        for i in range(2):
            tc = slice(i * 128, (i + 1) * 128)
            xs3 = v3(xs_tok[:, i, :])
            for gi in range(2):
                gc = slice(gi * 192, (gi + 1) * 192)
                self.mm(PS[2][:, gc], CT[:, gi, tc], Hb[:, gc], r=[("CT", p, gi), "Hb"], w=[k2])
                self.mm(PS[3][:, gc], B_tok[:, i, gi, :], xdtd[:, i, gc], r=[("Btok", p), "xdtd"], w=[k3])
            self.tt("dve", v3(yo[:, :]), v3(H[:, :]), b3(cdec[:, i * 6:i * 6 + 6]), ALU.mult, r=["H", "cdec"], w=["yo"])
            self.tt("dve", H[:, :], yo[:, :], PS[3][:, 0:384], ALU.add, r=["yo", k3], w=["H"])
            self.tt("dve", v3(yy[:, :]), v3(PS[2][:, 0:384]), b3(ea[:, i * 6:i * 6 + 6]), ALU.mult, r=[k2, "ea"], w=["yy"])
            self.cp("act", Hb[:, :], H[:, :], r=["H", k2], w=["Hb"])
            self.tt("dve", yy[:, :], yy[:, :], PS[6 + i][:, 0:384], ALU.add, r=["yy", yk[i]], w=["yy"])
            self.tt("dve", v3(yo[:, :]), xs3, b3(sp6[:, 2, :]), ALU.mult, r=[("xs", p, i), "dsk"], w=["yo"])
            self.tt("dve", yy[:, :], yy[:, :], yo[:, :], ALU.add, r=["yy", "yo"], w=["yy"])
            self.tt("dve", yy[:, :], yy[:, :], sz[:, i, :], ALU.mult, r=["yy", ("sz", p, i)], w=["yy"])
            self.memset("dve", ssq[:, 2:4], 0.0, w=["ssq2"])
            for gi in range(2):
                gc = slice(gi * 192, (gi + 1) * 192)
                self.act(yo[:, gc], yy[:, gc], AF.Square, accum=ssq[:, 2 + gi:3 + gi], r=["yy", "ssq2"], w=["yo", "ssq2"])
            self.rstd_from(ssq[:, 4:6], ssq[:, 2:4], 1.0 / 192.0, ["ssq2"], "ssq4")
            for gi in range(2):
                gc = slice(gi * 192, (gi + 1) * 192)
                self.stt("dve", mixp[:, i, 384 + gi * 192:384 + (gi + 1) * 192], yy[:, gc], ssq[:, 4 + gi:5 + gi],
                         normg[:, 384 + gi * 192:384 + (gi + 1) * 192], ALU.mult, ALU.mult,
                         r=["yy", "ssq4", "normg"], w=[("mixp", "ssd", i)])

    def attention_chunk(self, l, c, p, B):
        S = self.S
        PS = self.psum
        kT, qT, Vaug, ksum_b = B["kT"], B["qT"], B["Vaug"], B["ksum_b"]
        st12b, negM, ET = B["st12b"], B["negM"], B["ET"]
        gtmp, gtop, att_pre, rden, ssq, normg, mixp, sqt = (B["gtmp"], B["gtop"], B["att_pre"], B["rden"],
                                                                  B["ssq"], B["normg"], B["mixp"], B["sqt"])
        k5 = ("ps", 4)
        self.cp("dve", st12b[:, 0:6], B["qn2max"][:, :], r=[("qn2max", p)], w=["st12b"])
        self.cp("dve", st12b[:, 6:12], B["kn2max"][:, :], r=["kn2max"], w=["st12b"])
        self.tr(PS[4][0:12, 128:256], st12b[:, 0:12], self.ident_f, r=["st12b", "cstf"], w=[k5])
        S.add("dve", lambda e: e.reduce_max(out=negM[0:12, 7:8], in_=PS[4][0:12, 128:256], axis=AX.X), r=[k5], w=["redc"])
        self.ts("dve", B["dgt"][0:12, 0:12], self.ident_f[0:12, 0:12], negM[0:12, 7:8], None, ALU.mult, r=["redc", "cstf"], w=["dg"])
        self.mm(PS[4][:, 256:268], self.ones_f[0:12, :], B["dgt"][0:12, 0:12], r=["dg", "cstf"], w=[k5])
        self.cp("dve", st12b[:, 0:12], PS[4][:, 256:268], r=[k5], w=["st12b"])
        self.tt("dve", negM[:, 0:6], st12b[:, 0:6], st12b[:, 6:12], ALU.mult, r=["st12b"], w=["negM"])
        self.tt("pool", negM[:, 0:6], negM[:, 0:6], self.cstf[:, 713:714].to_broadcast([128, 6]), ALU.pow, r=["negM", "cstf"], w=["negM"])
        self.ts("dve", negM[:, 0:6], negM[:, 0:6], -1.0, None, ALU.mult, r=["negM"], w=["negM"])
        use_sel = c >= 4
        gsel = B["gsel"]
        if use_sel:
            for i in range(2):
                g5 = PS[4][:, 0:48].rearrange("p (h n) -> p h n", h=6)
                for h in range(6):
                    rows = slice((h % 2) * 64, (h % 2) * 64 + 64)
                    self.mm(g5[:, h, :], qT[rows, h // 2, i * 128:(i + 1) * 128], ksum_b[rows, h // 2, :],
                            r=[("qT", p, i), "ksum_b"], w=[k5])
                self.tt("dve", gtmp[:, :, :], g5, self.pastneg[:, c:c + 1, :].to_broadcast([128, 6, 8]), ALU.add,
                        r=[k5, "cstf"], w=["gtmp"])
                for h in range(6):
                    S.add("dve", (lambda h: lambda e: e.max(out=gtop[:, h, :], in_=gtmp[:, h, :]))(h), r=["gtmp"], w=["gtop"])
                self.tt("dve", gsel[:, i, :, :], gtmp[:, :, :], gtop[:, :, 2:3].to_broadcast([128, 6, 8]), ALU.is_ge,
                        r=["gtmp", "gtop"], w=["gsel"])
        nst = getattr(self, "_nst", 0)
        acc = B["acc"]
        atmp = B["atmp"]

        def s_and_exp(h, kt, q0):
            nonlocal nst
            rows = slice((h % 2) * 64, (h % 2) * 64 + 64)
            N = 256 - q0
            sb_ = 4 + nst % 2
            sk = ("ps", sb_)
            stp = PS[sb_][:, 0:N]
            self.mm(stp, kT[rows, h // 2, kt * 128:(kt + 1) * 128], qT[rows, h // 2, q0:256], start=True, stop=True,
                    r=[("kT", kt), ("qT", p, 0), ("qT", p, 1)], w=[sk])
            et = ET[nst % len(ET)]
            ek = ("ET", nst % len(ET))
            nst += 1
            self.act(et[:, 0:N], stp, AF.Exp, bias=negM[:, h:h + 1], r=[sk, "negM"], w=[ek])
            if kt >= 2 * c:
                self.tt("dve", et[:, 0:128], et[:, 0:128], self.tri_b, ALU.mult, r=[ek, "cstb1"], w=[ek])
            return et, ek

        nob = getattr(self, "_nob", 0)
        steps = []
        for h in range(6):
            if use_sel:
                kgroups = [([2 * c, 2 * c + 1], None)] + [([2 * n, 2 * n + 1], n) for n in range(c)]
            else:
                kgroups = [(list(range(2 * c + 2)), None)]
            for gi_, (kts, nsel) in enumerate(kgroups):
                ob = nob % 2
                nob += 1
                for kt in kts:
                    steps.append(dict(h=h, kt=kt, ob=ob, first=(kt == kts[0]), lastk=(kt == kts[-1]), nsel=nsel,
                                      lastg=(gi_ == len(kgroups) - 1)))

        def pv_and_post(st, et, ek):
            h, kt, ob = st["h"], st["kt"], st["ob"]
            okl = [("ps", 6), ("ps", 7)] if ob == 0 else [("ps", 2), ("ps", 3)]
            O = (self.ps67 if ob == 0 else self.ps23)[:, :].rearrange("p (b x) -> p b x", b=2)[:, :, 0:65]
            if kt <= 2 * c:
                q0, qtiles = 0, [0, 1]
            else:
                q0, qtiles = 128, [1]
            first = st["first"]
            for qi in qtiles:
                e0 = qi * 128 - q0
                self.mm(O[:, qi, :], et[:, e0:e0 + 128], Vaug[:, kt, h * 65:(h + 1) * 65], start=first,
                        stop=(st["lastk"] or (kt == 2 * c and qi == 0)), r=[ek, ("V", kt)], w=[okl[qi]])
            if not st["lastk"]:
                return
            nsel = st["nsel"]
            if not use_sel:
                src = O
                sk_ = list(okl)
            elif nsel is None:
                self.cp("dve", acc[:, :, :], O, r=okl, w=["acc"])
                return
            else:
                selb = gsel[:, :, h, nsel].unsqueeze(2).to_broadcast([128, 2, 65])
                self.tt("dve", atmp[:, :, :], O, selb, ALU.mult, r=okl + ["gsel"], w=["atmp"])
                self.tt("dve", acc[:, :, :], acc[:, :, :], atmp[:, :, :], ALU.add, r=["acc", "atmp"], w=["acc"])
                if not st["lastg"]:
                    return
                src = acc
                sk_ = ["acc"]
            S.add("dve", (lambda src: lambda e: e.reciprocal(out=rden[:, 0:2], in_=src[:, :, 64]))(src), r=sk_, w=["rden"])
            self.tt("dve", att_pre[:, :, h * 64:(h + 1) * 64], src[:, :, 0:64],
                    rden[:, 0:2].unsqueeze(2).to_broadcast([128, 2, 64]), ALU.mult, r=sk_ + ["rden"], w=["attpre"])

        LOOK = 2
        pend = []
        for st in steps:
            q0 = 0 if st["kt"] <= 2 * c else 128
            et, ek = s_and_exp(st["h"], st["kt"], q0)
            pend.append((st, et, ek))
            if len(pend) > LOOK:
                pv_and_post(*pend.pop(0))
            yield
        while pend:
            pv_and_post(*pend.pop(0))
        yield
        self._nob = nob
        self._nst = nst
        self.memset("dve", ssq[:, 6:8], 0.0, w=["ssq6"])
        for qi in range(2):
            self.act(sqt[:, :], att_pre[:, qi, :], AF.Square, accum=ssq[:, 6 + qi:7 + qi], r=["attpre", "ssq6"], w=["yo", "ssq6"])
        self.rstd_from(rden[:, 2:4], ssq[:, 6:8], 1.0 / 384.0, ["ssq6"], "rden2")
        for qi in range(2):
            self.stt("dve", mixp[:, qi, 0:384], att_pre[:, qi, :], rden[:, 2 + qi:3 + qi], normg[:, 0:384], ALU.mult, ALU.mult,
                     r=["attpre", "rden2", "normg"], w=[("mixp", "att", qi)])

    def ffn_layout(self):
        self.arena_reset()
        A = self.aalloc
        self.PB = A([128, 2, D], F32)
        self.hb2 = [A([128, D], BF16) for _ in range(2)]
        self.wp = [A([128, 4096], BF16) for _ in range(6)]
        self.actT = [A([128, 4, 512], BF16) for _ in range(2)]
        self.sg = [A([128, 512], F32) for _ in range(2)]
        self.gates = A([128, NT, 8], F32)
        self.Wr = A([128, 8, 8], BF16)
        self.rt_lg = A([128, 8], F32)
        self.rt_top = A([128, 8], F32)
        self.rt_p = A([128, 4], F32)
        self.rt_g = A([128, 2, 8], F32)
        self.slice_n = 0

    def wout_ln1(self, l):
        self.ffn_layout()
        PS = self.psum
        self.load_ln_params(self.ln1_g[l, :], self.ln1_b[l, :])
        for half in range(2):
            self.dma(self.wp[half][:, :].rearrange("p (kc n) -> p kc n", kc=8),
                     self.w_out[l, :, half * 512:(half + 1) * 512].rearrange("(kc p) n -> p kc n", p=128),
                     w=[("wp", half)], eng="pool", lane="wp%d" % half)
        banks = [0, 1, 2, 5]
        pend = None
        for t in range(NT):
            for half in range(2):
                bk = banks[(2 * t + half) % 4]
                pk = ("ps", bk)
                ws = self.wp[half][:, :].rearrange("p (kc n) -> p kc n", kc=8)
                for kc in range(8):
                    self.mm(PS[bk][:, 0:512], self.XT[:, kc, t * 128:(t + 1) * 128], ws[:, kc, :], start=(kc == 0), stop=(kc == 7),
                            r=[("XT", t, kc), ("wp", half)], w=[pk])
                hs = self.h_acc[:, t, half * 512:(half + 1) * 512]
                self.tt("dve", hs, hs, PS[bk][:, 0:512], ALU.add, r=[("h", t), pk], w=[("h", t)])
            pn = self.ln_tile(t, final=False, defer=True)
            if pend is not None:
                pend()
            pend = pn
        if pend is not None:
            pend()

    def ffn_core(self, wg, wu, wd, dff, gate_of):
        PS = self.psum
        nsl = (dff + 511) // 512
        for s_ in range(nsl):
            f0 = s_ * 512
            fw = min(512, dff - f0)
            nfc = fw // 128
            base = (self.slice_n % 2) * 3
            self.slice_n += 1
            sg_, su_, sd_ = base, base + 1, base + 2
            wgv = self.wp[sg_][:, 0:8 * fw].rearrange("p (kc n) -> p kc n", kc=8)
            wuv = self.wp[su_][:, 0:8 * fw].rearrange("p (kc n) -> p kc n", kc=8)
            wdv = self.wp[sd_][:, 0:nfc * 1024].rearrange("p (fc n) -> p fc n", fc=nfc)
            self.dma(wgv, wg[:, f0:f0 + fw].rearrange("(kc p) n -> p kc n", p=128), w=[("wp", sg_)], eng="pool", lane="wp%d" % sg_)
            self.dma(wuv, wu[:, f0:f0 + fw].rearrange("(kc p) n -> p kc n", p=128), w=[("wp", su_)], eng="pool", lane="wp%d" % su_)
            self.dma(wdv, wd[f0:f0 + fw, :].rearrange("(fc p) n -> p fc n", p=128), w=[("wp", sd_)], eng="pool", lane="wp%d" % sd_)
            for tcx in range(4):
                ab = self.ab_n % 2
                self.ab_n += 1
                aT = self.actT[ab]
                cols = slice(tcx * 512, (tcx + 1) * 512)
                xk = lambda kc: [("XT", t, kc) for t in range(4 * tcx, 4 * tcx + 4)]
                for fc in range(nfc):
                    gb, ub = fc % 2, 2 + fc % 2
                    for kc in range(8):
                        self.mm(PS[gb][:, 0:512], wgv[:, kc, fc * 128:(fc + 1) * 128], self.XT[:, kc, cols], start=(kc == 0),
                                stop=(kc == 7), r=xk(kc) + [("wp", sg_)], w=[("ps", gb)])
                    for kc in range(8):
                        self.mm(PS[ub][:, 0:512], wuv[:, kc, fc * 128:(fc + 1) * 128], self.XT[:, kc, cols], start=(kc == 0),
                                stop=(kc == 7), r=xk(kc) + [("wp", su_)], w=[("ps", ub)])
                    sgb = self.sg[fc % 2]
                    self.act(sgb[:, :], PS[gb][:, 0:512], AF.Silu, r=[("ps", gb)], w=[("sg", fc % 2)])
                    self.tt("dve", aT[:, fc, :], sgb[:, :], PS[ub][:, 0:512], ALU.mult, r=[("sg", fc % 2), ("ps", ub)], w=[("actT", ab)])
                def down_fn(tcx=tcx, aT=aT, ab=ab, wdv=wdv, sd_=sd_, nfc=nfc):
                    for i in range(4):
                        t = 4 * tcx + i
                        for half in range(2):
                            db = 4 + (2 * i + half) % 4
                            for fc in range(nfc):
                                self.mm(PS[db][:, 0:512], aT[:, fc, i * 128:(i + 1) * 128], wdv[:, fc, half * 512:(half + 1) * 512],
                                        start=(fc == 0), stop=(fc == nfc - 1), r=[("actT", ab), ("wp", sd_)], w=self.pkeys(db))
                            hs = self.h_acc[:, t, half * 512:(half + 1) * 512]
                            g = gate_of(t)
                            if g is None:
                                self.tt("dve", hs, hs, PS[db][:, 0:512], ALU.add, r=[("h", t)] + self.pkeys(db), w=[("h", t)])
                            else:
                                self.stt("dve", hs, PS[db][:, 0:512], g, hs, ALU.mult, ALU.add, r=[("h", t), "gates"] + self.pkeys(db), w=[("h", t)])
                if self.pend_down is not None:
                    self.pend_down()
                self.pend_down = down_fn

    def flush_down(self):
        if getattr(self, "pend_down", None) is not None:
            self.pend_down()
        self.pend_down = None

    def ffn(self, l):
        self.ab_n = 0
        self.pend_down = None
        self.ffn_core(self.ffn_wg[l // 2, :, :], self.ffn_wu[l // 2, :, :], self.ffn_wd[l // 2, :, :], D_FF, lambda t: None)
        self.flush_down()

    def moe(self, l):
        S = self.S
        PS = self.psum
        self.ab_n = 0
        self.pend_down = None
        m = l // 2
        self.dma(self.Wr[:, :, :], self.router_w[m, :, :].rearrange("(kc p) e -> p kc e", p=128), w=["Wr"], eng="pool", lane="wp5")
        lg, top, p, g2 = self.rt_lg, self.rt_top, self.rt_p, self.rt_g
        for t in range(NT):
            pk = ("ps", 5)
            for kc in range(8):
                self.mm(PS[5][:, 0:8], self.XT[:, kc, t * 128:(t + 1) * 128], self.Wr[:, kc, :], start=(kc == 0), stop=(kc == 7),
                        r=[("XT", t, kc), "Wr"], w=[pk])
            self.cp("dve", lg[:, :], PS[5][:, 0:8], r=[pk], w=["rt_lg"])
            S.add("dve", lambda e: e.max(out=top[:, :], in_=lg[:, :]), r=["rt_lg"], w=["rt_top"])
            self.tt("dve", p[:, 0:1], top[:, 1:2], top[:, 0:1], ALU.subtract, r=["rt_top"], w=["rt_p"])
            self.act(p[:, 1:2], p[:, 0:1], AF.Sigmoid, r=["rt_p"], w=["rt_p2"])
            self.ts("dve", p[:, 2:3], p[:, 1:2], -1.0, 1.0, ALU.mult, ALU.add, r=["rt_p2"], w=["rt_p1"])
            self.ts("dve", g2[:, 0, :], lg[:, :], top[:, 0:1], p[:, 2:3], ALU.is_equal, ALU.mult, r=["rt_lg", "rt_top", "rt_p1"], w=["rt_g0"])
            self.ts("dve", g2[:, 1, :], lg[:, :], top[:, 1:2], p[:, 1:2], ALU.is_equal, ALU.mult, r=["rt_lg", "rt_top", "rt_p2"], w=["rt_g1"])
            self.tt("dve", self.gates[:, t, :], g2[:, 0, :], g2[:, 1, :], ALU.add, r=["rt_g0", "rt_g1"], w=["gates"])
        self.dump("gates", self.gates[:, :, :], r=["gates"])
        nexp = getattr(self, "n_experts", NEXP)
        for e_ in range(nexp):
            self.ffn_core(self.moe_wg[m, e_, :, :], self.moe_wu[m, e_, :, :], self.moe_wd[m, e_, :, :], D_FFE,
                          (lambda e_: lambda t: self.gates[:, t, e_:e_ + 1])(e_))
        self.flush_down()


def _consts():
    c = np.zeros((128, 848), np.float32)
    i = np.arange(128)
    c[:, 0:128] = np.eye(128, dtype=np.float32)
    c[:, 128:256] = (i[:, None] <= i[None, :]).astype(np.float32)
    c[:, 256:384] = np.where(i[None, :] >= i[:, None], 0.0, -BIG)
    c[:, 384:512] = (i[None, :] <= i[:, None]).astype(np.float32)
    c[:, 512:640] = 1.0
    pn = np.where(np.arange(8)[None, :] < np.arange(8)[:, None], 0.0, -1e30).astype(np.float32)
    c[:, 640:704] = pn.reshape(1, 64)
    inv = (500000.0 ** (-np.arange(0, 16, 2, dtype=np.float32) / 16.0)).astype(np.float32)
    c[:, 704:712] = inv[None, :]
    c[:, 712] = -0.5
    c[:, 713] = 0.5
    oneh = np.zeros((8, 8, 128), np.float32)
    for n in range(8):
        oneh[n, n, :] = 1.0
    return c, oneh.reshape(8, 1024)


_CACHE = {}


def _prep_shared(inputs):
    f = lambda a: np.ascontiguousarray(np.asarray(a, dtype=np.float32))
    cst, oneh = _consts()
    sh = {
        "cst": cst, "oneh": oneh,
        "ln_in_g": f(inputs["ln_in_g"]), "ln_in_b": f(inputs["ln_in_b"]),
        "w_in": f(inputs["w_in"]),
        "conv_wT": f(np.asarray(inputs["conv_w"]).reshape(DEPTH, 4, 7, 128).transpose(0, 3, 2, 1)),
        "conv_bT": f(np.asarray(inputs["conv_b"]).reshape(DEPTH, 7, 128).transpose(0, 2, 1)),
        "dt_bias": f(inputs["dt_bias"]), "a_log": f(inputs["a_log"]), "d_skip": f(inputs["d_skip"]),
        "normg": f(np.concatenate([np.asarray(inputs["att_norm_g"]), np.asarray(inputs["ssd_norm_g"]),
                                   np.asarray(inputs["gm_norm_g"])], axis=1)),
        "gm_ln_g": f(inputs["gm_ln_g"]), "gm_ln_b": f(inputs["gm_ln_b"]),
        "gm_w_s": f(inputs["gm_w_s"]),
        "gm_b_sT": f(np.asarray(inputs["gm_b_s"]).transpose(0, 2, 1)),
        "w_out": f(inputs["w_out"]),
        "ln1_g": f(inputs["ln1_g"]), "ln1_b": f(inputs["ln1_b"]),
        "ln2_g": f(inputs["ln2_g"]), "ln2_b": f(inputs["ln2_b"]),
        "ffn_w_gate": f(inputs["ffn_w_gate"]), "ffn_w_up": f(inputs["ffn_w_up"]), "ffn_w_down": f(inputs["ffn_w_down"]),
        "router_w": f(inputs["router_w"]),
        "moe_w_gate": f(inputs["moe_w_gate"]), "moe_w_up": f(inputs["moe_w_up"]), "moe_w_down": f(inputs["moe_w_down"]),
    }
    return sh


def run(inputs, cores=8, dbg=(), stop=None, n_layers=DEPTH, trace=False):
    b = Builder(n_layers=n_layers, dbg=dbg, stop=stop)
    nc = b.build()
    sh = _prep_shared(inputs)
    x = np.asarray(inputs["x"], dtype=np.float32)
    pos = np.asarray(inputs["positions"]).astype(np.int32)
    in_maps = []
    for c in range(cores):
        m = dict(sh)
        m["x"] = np.ascontiguousarray(x[c])
        m["pos"] = np.ascontiguousarray(pos[c].reshape(NT, 128).T)
        in_maps.append(m)
    res = run_bass_kernel_spmd(nc, in_maps, core_ids=list(range(cores)), trace=trace)
    return res, b


def kernel(**inputs):
    res, b = run(inputs)
    out = np.stack([np.asarray(r["y"]) for r in res.results], axis=0)
    return out.astype(np.float32)
```
